# Optimizing a Trainium2 kernel written in Bass

```python
import jax, jax.numpy as jnp
from jax import lax
import numpy as np

D_MODEL = 1024
BATCH = 4
SEQ = 4096
DEPTH = 2

GLA_HEADS = 4
GLA_DK = 64
GLA_DV = 128
GLA_GATE_RANK = 16
GLA_GATE_TAU = 16.0
GLA_CHUNK = 64
DSA_HEADS = 8
DSA_HD = 64
IDX_HEADS = 4
IDX_HD = 64
TOPK_MAX = 256
Q_BLOCK = 128
ROPE_THETA = 500000.0
ROPE_FRAC_DIV = 4
SGU_CHUNK = 128
SGU_GROUPS = 8
SGU_WIDTH = D_MODEL
D_FF = 2816
CONV_W = 3
EPS = 1e-6
LN_EPS = 1e-5

A_WIDTH = GLA_HEADS * GLA_DV
B_WIDTH = DSA_HEADS * DSA_HD
IN_WIDTHS = (
    GLA_HEADS * GLA_DK,
    GLA_HEADS * GLA_DK,
    GLA_HEADS * GLA_DV,
    A_WIDTH,
    GLA_GATE_RANK,
    DSA_HEADS * DSA_HD,
    DSA_HD,
    DSA_HD,
    IDX_HEADS * IDX_HD,
    IDX_HD,
    IDX_HEADS,
)
IN_WIDTH = sum(IN_WIDTHS)
N_EVEN = (DEPTH + 1) // 2
N_ODD = DEPTH // 2

kernel_name = "hybrid_gla_dsa_sgu_convffn"


def rmsnorm(x, g):
    xf = x.astype(jnp.float32)
    y = xf * lax.rsqrt(jnp.mean(xf * xf, axis=-1, keepdims=True) + EPS)
    return (y * g.astype(jnp.float32)).astype(x.dtype)


def layernorm(x, g, b):
    xf = x.astype(jnp.float32)
    mu = jnp.mean(xf, axis=-1, keepdims=True)
    xc = xf - mu
    y = xc * lax.rsqrt(jnp.mean(xc * xc, axis=-1, keepdims=True) + LN_EPS)
    return (y * g.astype(jnp.float32) + b.astype(jnp.float32)).astype(x.dtype)


def split_cols(a, widths):
    out, start = [], 0
    for w in widths:
        out.append(a[..., start:start + w])
        start += w
    return out


def rope_partial(x, pos):
    d = x.shape[-1]
    rd = d // ROPE_FRAC_DIV
    half = rd // 2
    inv = jnp.power(ROPE_THETA, -(jnp.arange(half, dtype=jnp.float32) * 2.0 / rd))
    ang = pos[:, None] * inv[None, :]
    cos = jnp.cos(ang)[:, None, :].astype(x.dtype)
    sin = jnp.sin(ang)[:, None, :].astype(x.dtype)
    x1, x2, rest = x[..., :half], x[..., half:rd], x[..., rd:]
    return jnp.concatenate([x1 * cos - x2 * sin, x2 * cos + x1 * sin, rest], axis=-1)


def gla_chunked(q, k, v, log_a):
    Bn, T, H, dk = q.shape
    dv = v.shape[-1]
    C = GLA_CHUNK
    N = T // C

    def chunks(a):
        return a.astype(jnp.float32).reshape(Bn, N, C, H, a.shape[-1]).transpose(0, 3, 1, 2, 4)

    qf = chunks(q) * (dk ** -0.5)
    kf, vf, la = chunks(k), chunks(v), chunks(log_a)
    b = jnp.cumsum(la, axis=3)
    b_last = b[:, :, :, -1:]
    b_mid = b[:, :, :, C // 2:C // 2 + 1]
    att = jnp.einsum('bhnid,bhnjd->bhnij', qf * jnp.exp(b - b_mid), kf * jnp.exp(b_mid - b))
    att = att * jnp.tril(jnp.ones((C, C), jnp.float32))
    o_intra = jnp.einsum('bhnij,bhnjd->bhnid', att, vf)
    dS = jnp.einsum('bhncd,bhnce->bhnde', kf * jnp.exp(b_last - b), vf)
    decay = jnp.exp(b_last[:, :, :, 0])

    def step(S, inp):
        dec, ds = inp
        return dec[..., None] * S + ds, S

    S0 = jnp.zeros((Bn, H, dk, dv), jnp.float32)
    _, S_prev = lax.scan(step, S0, (jnp.moveaxis(decay, 2, 0), jnp.moveaxis(dS, 2, 0)))
    S_prev = jnp.moveaxis(S_prev, 0, 2)
    o_inter = jnp.einsum('bhncd,bhnde->bhnce', qf * jnp.exp(b), S_prev)
    o = o_intra + o_inter
    return o.transpose(0, 2, 3, 1, 4).reshape(Bn, T, H, dv)


def dsa_attention(q, k, v, iq, ik, iw, topk):
    Bn, T, Hq, d = q.shape
    di = ik.shape[-1]
    NB = T // Q_BLOCK
    key_pos = jnp.arange(T, dtype=jnp.int32)
    ikf = ik.astype(jnp.float32)

    def to_blocks(a):
        return jnp.moveaxis(a.reshape((Bn, NB, Q_BLOCK) + a.shape[2:]), 1, 0)

    q_pos = key_pos.reshape(NB, Q_BLOCK)

    def one_block(args):
        qb, iqb, iwb, qp = args
        s_idx = jnp.einsum('bqhd,bsd->bqhs', iqb.astype(jnp.float32), ikf) * (di ** -0.5)
        score = jnp.einsum('bqh,bqhs->bqs', iwb.astype(jnp.float32), jax.nn.relu(s_idx))
        causal = key_pos[None, :] <= qp[:, None]
        score = jnp.where(causal[None], score, -jnp.inf)
        _, idx = lax.top_k(score, topk)
        kg = jax.vmap(lambda kb, ib: kb[ib])(k, idx)
        vg = jax.vmap(lambda vb, ib: vb[ib])(v, idx)
        valid = idx <= qp[None, :, None]
        s = jnp.einsum('bqhd,bqkd->bqhk', qb.astype(jnp.float32), kg.astype(jnp.float32)) * (d ** -0.5)
        s = jnp.where(valid[:, :, None, :], s, -jnp.inf)
        p = jax.nn.softmax(s, axis=-1)
        o = jnp.einsum('bqhk,bqkd->bqhd', p, vg.astype(jnp.float32))
        return o.astype(q.dtype)

    ob = lax.map(one_block, (to_blocks(q), to_blocks(iq), to_blocks(iw), q_pos))
    return jnp.moveaxis(ob, 0, 1).reshape(Bn, T, Hq * d)


def hybrid_attn_layer(x, g, w_in, w_a2, b_a, head_g, w_o, topk):
    Bn, T, _ = x.shape
    pos = jnp.arange(T, dtype=jnp.float32)
    h = rmsnorm(x, g)
    proj = h @ w_in
    aq, ak, av, ar, alr, bq, bk, bv, iq, ik, iw = split_cols(proj, IN_WIDTHS)
    log_a = jax.nn.log_sigmoid((alr @ w_a2 + b_a).astype(jnp.float32)) / GLA_GATE_TAU
    oa = gla_chunked(aq.reshape(Bn, T, GLA_HEADS, GLA_DK),
                     ak.reshape(Bn, T, GLA_HEADS, GLA_DK),
                     av.reshape(Bn, T, GLA_HEADS, GLA_DV),
                     log_a.reshape(Bn, T, GLA_HEADS, GLA_DK))
    oa = rmsnorm(oa, head_g).reshape(Bn, T, A_WIDTH).astype(x.dtype) * jax.nn.silu(ar)
    bq = rope_partial(bq.reshape(Bn, T, DSA_HEADS, DSA_HD), pos)
    bk = rope_partial(bk[:, :, None, :], pos)[:, :, 0, :]
    iq = rope_partial(iq.reshape(Bn, T, IDX_HEADS, IDX_HD), pos)
    ik = rope_partial(ik[:, :, None, :], pos)[:, :, 0, :]
    iw = iw * (IDX_HEADS ** -0.5)
    ob = dsa_attention(bq, bk, bv, iq, ik, iw, topk)
    return x + jnp.concatenate([oa, ob], axis=-1) @ w_o


def sgu_layer(x, g, w_uv, ln_g, ln_b, w_s, b_s, w_out):
    Bn, T, _ = x.shape
    NC = T // SGU_CHUNK
    gw = SGU_WIDTH // SGU_GROUPS
    h = rmsnorm(x, g)
    z = jax.nn.gelu(h @ w_uv)
    u, v = z[..., :SGU_WIDTH], z[..., SGU_WIDTH:]
    v = layernorm(v, ln_g, ln_b)
    vc = v.reshape(Bn, NC, SGU_CHUNK, SGU_GROUPS, gw)
    ws = w_s * jnp.tril(jnp.ones((SGU_CHUNK, SGU_CHUNK), w_s.dtype))
    mixed = jnp.einsum('gts,bnsgc->bntgc', ws, vc) + b_s.T[None, None, :, :, None]
    return x + (u * mixed.reshape(Bn, T, SGU_WIDTH)) @ w_out


def conv_ffn(x, g, w_up, conv_w, conv_b, w_down):
    T = x.shape[1]
    h = rmsnorm(x, g)
    a = h @ w_up
    ap = jnp.pad(a, ((0, 0), (CONV_W - 1, 0), (0, 0)))
    c = conv_b + sum(conv_w[j] * ap[:, j:j + T] for j in range(CONV_W))
    gate, up = c[..., :D_FF], c[..., D_FF:]
    return x + (jax.nn.silu(gate) * up) @ w_down


def setup_inputs(seed: int = 0) -> dict:
    key = jax.random.key(seed)
    ks = jax.random.split(key, 24)

    def nrm(k, shape, scale):
        return jax.random.normal(k, shape, jnp.float32) * scale

    D = D_MODEL
    return {
        "x": nrm(ks[0], (BATCH, SEQ, D), 1.0),
        "attn_norm": 1.0 + nrm(ks[1], (N_EVEN, D), 0.02),
        "attn_w_in": nrm(ks[2], (N_EVEN, D, IN_WIDTH), D ** -0.5),
        "gla_w_a2": nrm(ks[3], (N_EVEN, GLA_GATE_RANK, GLA_HEADS * GLA_DK), GLA_GATE_RANK ** -0.5),
        "gla_b_a": nrm(ks[4], (N_EVEN, GLA_HEADS * GLA_DK), 0.1),
        "gla_head_g": 1.0 + nrm(ks[5], (N_EVEN, GLA_DV), 0.02),
        "attn_w_o": nrm(ks[6], (N_EVEN, A_WIDTH + B_WIDTH, D), (A_WIDTH + B_WIDTH) ** -0.5),
        "sgu_norm": 1.0 + nrm(ks[7], (N_ODD, D), 0.02),
        "sgu_w_uv": nrm(ks[8], (N_ODD, D, 2 * SGU_WIDTH), D ** -0.5),
        "sgu_ln_g": 1.0 + nrm(ks[9], (N_ODD, SGU_WIDTH), 0.02),
        "sgu_ln_b": nrm(ks[10], (N_ODD, SGU_WIDTH), 0.02),
        "sgu_w_s": nrm(ks[11], (N_ODD, SGU_GROUPS, SGU_CHUNK, SGU_CHUNK), 0.5 * SGU_CHUNK ** -0.5),
        "sgu_b_s": 1.0 + nrm(ks[12], (N_ODD, SGU_GROUPS, SGU_CHUNK), 0.02),
        "sgu_w_out": nrm(ks[13], (N_ODD, SGU_WIDTH, D), SGU_WIDTH ** -0.5),
        "ffn_norm": 1.0 + nrm(ks[14], (DEPTH, D), 0.02),
        "ffn_w_up": nrm(ks[15], (DEPTH, D, 2 * D_FF), D ** -0.5),
        "ffn_conv_w": nrm(ks[16], (DEPTH, CONV_W, 2 * D_FF), CONV_W ** -0.5),
        "ffn_conv_b": nrm(ks[17], (DEPTH, 2 * D_FF), 0.02),
        "ffn_w_down": nrm(ks[18], (DEPTH, D_FF, D), D_FF ** -0.5),
        "final_norm": 1.0 + nrm(ks[19], (D,), 0.02),
    }


def reference(x, attn_norm, attn_w_in, gla_w_a2, gla_b_a, gla_head_g, attn_w_o,
              sgu_norm, sgu_w_uv, sgu_ln_g, sgu_ln_b, sgu_w_s, sgu_b_s, sgu_w_out,
              ffn_norm, ffn_w_up, ffn_conv_w, ffn_conv_b, ffn_w_down, final_norm):
    T = x.shape[1]
    topk = min(TOPK_MAX, T // 4)
    for i in range(DEPTH):
        j = i // 2
        if i % 2 == 0:
            x = hybrid_attn_layer(x, attn_norm[j], attn_w_in[j], gla_w_a2[j], gla_b_a[j],
                                  gla_head_g[j], attn_w_o[j], topk)
        else:
            x = sgu_layer(x, sgu_norm[j], sgu_w_uv[j], sgu_ln_g[j], sgu_ln_b[j],
                          sgu_w_s[j], sgu_b_s[j], sgu_w_out[j])
        x = conv_ffn(x, ffn_norm[i], ffn_w_up[i], ffn_conv_w[i], ffn_conv_b[i], ffn_w_down[i])
    return rmsnorm(x, final_norm)
```

```python
import contextlib
import os
import numpy as np
import concourse.bass as bass
import concourse.mybir as mybir
from concourse.bass_utils import run_bass_kernel_spmd

F32 = mybir.dt.float32
BF16 = mybir.dt.bfloat16
I32 = mybir.dt.int32
AF = mybir.ActivationFunctionType
ALU = mybir.AluOpType
AX = mybir.AxisListType

ENGS = ("pe", "act", "dve", "pool", "sp")
NEG = -30000.0
NB = 32
OWN0 = 14
NOWN = NB - OWN0
D = 1024
DFF = 2816
NBIS = 16


class _Op:
    __slots__ = ("eng", "fn", "reads", "writes", "idx", "deps", "need_inc",
                 "milestone", "is_dma", "dsem", "dval", "dprev")


class Sched:
    NDMA = 8

    def __init__(self, nc):
        self.nc = nc
        self.ops = []
        self.last_w = {}
        self.readers = {}
        self.bar = set()
        self.mute = False

    def barrier(self):
        last = {}
        dl = {}
        for op in self.ops:
            if op.is_dma:
                dl.setdefault(op.eng, []).append(op.idx)
            else:
                last[op.eng] = op.idx
        bar = set(last.values())
        for e, l in dl.items():
            bar.update(l[-self.NDMA:])
        self.bar = bar

    def _add(self, eng, fn, reads, writes, is_dma):
        if self.mute:
            return None
        op = _Op()
        op.eng, op.fn, op.reads, op.writes, op.is_dma = eng, fn, tuple(reads), tuple(writes), is_dma
        op.need_inc = False
        op.milestone = 0
        op.dsem = None
        op.dval = 0
        op.dprev = 0
        op.idx = len(self.ops)
        deps = set()
        for k in op.reads:
            w = self.last_w.get(k)
            if w is not None:
                deps.add(w)
        for k in op.writes:
            w = self.last_w.get(k)
            if w is not None:
                deps.add(w)
            for r in self.readers.get(k, ()):
                deps.add(r)
        deps.update(self.bar)
        deps.discard(op.idx)
        op.deps = deps
        for k in op.reads:
            self.readers.setdefault(k, []).append(op.idx)
        for k in op.writes:
            self.last_w[k] = op.idx
            self.readers[k] = []
        self.ops.append(op)
        return op

    def pe(self, fn, reads=(), writes=()):
        return self._add("pe", fn, reads, writes, False)

    def act(self, fn, reads=(), writes=()):
        return self._add("act", fn, reads, writes, False)

    def dve(self, fn, reads=(), writes=()):
        return self._add("dve", fn, reads, writes, False)

    def dma(self, queue, fn, reads=(), writes=()):
        return self._add(queue, fn, reads, writes, True)

    @staticmethod
    def _hazard(p, op):
        return bool((set(p.writes) & (set(op.reads) | set(op.writes))) or (set(p.reads) & set(op.writes)))

    def emit(self):
        nc = self.nc
        ops = self.ops
        for op in ops:
            for d in op.deps:
                p = ops[d]
                if p.is_dma:
                    continue
                if p.eng == op.eng and not op.is_dma:
                    if p.eng == "pe" or not self._hazard(p, op):
                        continue
                p.need_inc = True
        per_eng = {e: [] for e in ENGS}
        for op in ops:
            per_eng[op.eng].append(op)
        dcnt = {}
        for e in ENGS:
            n = 0
            for op in per_eng[e]:
                if op.is_dma:
                    k = dcnt.get(e, 0)
                    dcnt[e] = k + 1
                    op.dsem = (e, k % self.NDMA)
                    op.dprev = 16 * (k // self.NDMA)
                    op.dval = op.dprev + 16
                elif op.need_inc:
                    n += 1
                    op.milestone = n
        with contextlib.ExitStack() as es:
            esem = {e: es.enter_context(nc.semaphore("ms_" + e)) for e in ENGS}
            dsem = {}
            for e in dcnt:
                for k in range(min(self.NDMA, dcnt[e])):
                    dsem[(e, k)] = es.enter_context(nc.semaphore("dq_%s_%d" % (e, k)))
            block = es.enter_context(nc.Block())

            def run(e, eng):
                seen = {}
                for op in per_eng[e]:
                    need = {}
                    for d in op.deps:
                        p = ops[d]
                        if p.is_dma:
                            need[p.dsem] = max(need.get(p.dsem, 0), p.dval)
                        else:
                            if not p.need_inc:
                                continue
                            if p.eng == e and not op.is_dma:
                                if e == "pe" or not self._hazard(p, op):
                                    continue
                            need[p.eng] = max(need.get(p.eng, 0), p.milestone)
                    if op.is_dma and op.dprev > 0:
                        need[op.dsem] = max(need.get(op.dsem, 0), op.dprev)
                    for key, val in need.items():
                        if seen.get(key, 0) >= val:
                            continue
                        seen[key] = val
                        eng.wait_ge(dsem[key] if isinstance(key, tuple) else esem[key], val)
                    ins = op.fn(eng)
                    if op.is_dma:
                        ins.then_inc(dsem[op.dsem], 16)
                    elif op.need_inc:
                        ins.then_inc(esem[e], 1)
                if e in dcnt:
                    last = {}
                    for op in per_eng[e]:
                        if op.is_dma:
                            last[op.dsem] = op.dval
                    for key, val in last.items():
                        if seen.get(key, 0) < val:
                            eng.wait_ge(dsem[key], val)

            if per_eng["pe"]:
                block.tensor(lambda eng: run("pe", eng))
            if per_eng["act"]:
                block.scalar(lambda eng: run("act", eng))
            if per_eng["dve"]:
                block.vector(lambda eng: run("dve", eng))
            if per_eng["pool"]:
                block.gpsimd(lambda eng: run("pool", eng))
            if per_eng["sp"]:
                block.sync(lambda eng: run("sp", eng))
        return {e: len(per_eng[e]) for e in ENGS}


class B:
    ARENA_WORDS = 53000

    def __init__(self, nc):
        self.nc = nc
        self.S = Sched(nc)
        self.n = 0
        self.arena = nc.alloc_sbuf_tensor("arena", [128, self.ARENA_WORDS], F32).ap()
        self.off = 0
        self.mark = 0

    def sb(self, name, shape, dt):
        esz = {F32: 4, BF16: 2, I32: 4}[dt]
        nel = int(np.prod(shape[1:]))
        words = (nel * esz + 31) // 32 * 8
        assert self.off + words <= self.ARENA_WORDS, "SBUF arena overflow at %s: %d" % (name, self.off + words)
        ap = self.arena[0:shape[0], self.off:self.off + words]
        if not hasattr(self, "names"):
            self.names = {}
        self.names[name] = (self.off, tuple(shape), dt)
        self.off += words
        if dt != F32:
            ap = ap.bitcast(dt)
        ap = ap[:, 0:nel]
        if len(shape) == 3:
            ap = ap.rearrange("p (a b) -> p a b", b=shape[2])
        elif len(shape) == 4:
            ap = ap.rearrange("p (a b c) -> p a b c", b=shape[2], c=shape[3])
        return ap

    def persist(self):
        self.mark = self.off

    def new_stage(self):
        self.off = self.mark
        self.S.barrier()

    def ps(self, name, shape, dt):
        self.n += 1
        return self.nc.alloc_psum_tensor("%s_%d" % (name, self.n), list(shape), dt).ap()

    def mm(self, out, lhsT, rhs, start, stop, r, w):
        self.S.pe(lambda e: e.matmul(out, lhsT=lhsT, rhs=rhs, start=start, stop=stop, skip_group_check=True), r, w)

    def tr(self, out, in_, ident, r, w):
        self.S.pe(lambda e: e.transpose(out=out, in_=in_, identity=ident), list(r) + ["ident", "identf"], w)

    def act(self, out, in_, func, r, w, bias=None, scale=None, accum_out=None):
        kw = {}
        if bias is not None:
            kw["bias"] = bias
        if scale is not None:
            kw["scale"] = scale
        if accum_out is not None:
            kw["accum_out"] = accum_out
        self.S.act(lambda e: e.activation(out=out, in_=in_, func=func, **kw), r, w)

    def ts(self, out, in0, s1, s2, op0, op1, r, w, accum_out=None):
        kw = {}
        if accum_out is not None:
            kw["accum_out"] = accum_out
        if op1 is None:
            self.S.dve(lambda e: e.tensor_scalar(out=out, in0=in0, scalar1=s1, scalar2=None, op0=op0, **kw), r, w)
        else:
            self.S.dve(lambda e: e.tensor_scalar(out=out, in0=in0, scalar1=s1, scalar2=s2, op0=op0, op1=op1, **kw), r, w)

    def tt(self, out, in0, in1, op, r, w):
        self.S.dve(lambda e: e.tensor_tensor(out=out, in0=in0, in1=in1, op=op), r, w)

    def stt(self, out, in0, scalar, in1, op0, op1, r, w):
        self.S.dve(lambda e: e.scalar_tensor_tensor(out=out, in0=in0, scalar=scalar, in1=in1, op0=op0, op1=op1), r, w)

    def cpv(self, out, in_, r, w):
        self.S.dve(lambda e: e.tensor_copy(out=out, in_=in_), r, w)

    def cpa(self, out, in_, r, w):
        self.S.act(lambda e: e.copy(out=out, in_=in_), r, w)

    def memset(self, ap, val, w):
        self.S.dve(lambda e: e.memset(ap, val), (), w)

    def recip(self, out, in_, r, w):
        self.S.dve(lambda e: e.reciprocal(out=out, in_=in_), r, w)

    def dma(self, q, out, in_, r, w):
        self.S.dma(q, lambda e: e.dma_start(out=out, in_=in_), r, w)

    def rmsnorm(self, x, xk, gbc, gk, h, hk, st, stk, junk, junkk, n=D, eps=1e-6):
        self.act(junk, x, AF.Square, [xk], [junkk, stk], accum_out=st[:, 0:1])
        self.ts(st[:, 1:2], st[:, 0:1], 1.0 / n, eps, ALU.mult, ALU.add, [stk], [stk])
        self.act(st[:, 2:3], st[:, 1:2], AF.Sqrt, [stk], [stk])
        self.recip(st[:, 3:4], st[:, 2:3], [stk], [stk])
        self.stt(h, x, st[:, 3:4], gbc, ALU.mult, ALU.mult, [xk, stk, gk], [hk])


def load_w(b, q, dst, src2d, c0, c1, d0, wkey):
    v = src2d.rearrange("(k p) n -> p k n", p=128)
    b.dma(q, dst[:, :, d0:d0 + (c1 - c0)], v[:, :, c0:c1], [], [wkey])


def stage_ffn(b, C, li, src, dst, ob0, nblk, final, tag, sk, dk):
    nc = b.nc
    T = lambda s: "%s_%s" % (tag, s)
    w_up, conv_w, conv_b, w_dn, gnorm = C["ffn_w_up"][li], C["ffn_conv_w"][li], C["ffn_conv_b"][li], C["ffn_w_down"][li], C["ffn_norm"][li]
    ident, identf = C["ident"], C["identf"]
    PA, PB, PC, PD = C["PA"], C["PB"], C["PC"], C["PD"]
    wdn = b.sb("wdn", [128, 22, D], BF16)
    b.dma("pool", wdn, w_dn.rearrange("(k p) n -> p k n", p=128), [], [T("wdn")])
    gbc = b.sb("gbc", [128, D], F32)
    b.dma("sp", gbc, gnorm.partition_broadcast(128), [], [T("gbc")])
    raw = b.sb("cwraw", [44, 4, 128], F32)
    for j in range(3):
        b.dma("sp", raw[:, j, :], conv_w[j].rearrange("(c p) -> c p", p=128), [], [T("raw")])
    b.dma("sp", raw[:, 3, :], conv_b.rearrange("(c p) -> c p", p=128), [], [T("raw")])
    cwb = b.sb("cwb", [128, 4, 44], F32)
    pv = PB[:, 0:176].rearrange("p (a c) -> p a c", c=44)
    for j in range(4):
        b.tr(pv[:, j, :], raw[:, j, :], identf[0:44, 0:44], [T("raw")], ["P2"])
    b.cpv(cwb, pv, ["P2"], [T("cwb")])
    if final:
        fbc = b.sb("fbc", [128, D], F32)
        b.dma("sp", fbc, C["final_norm"].partition_broadcast(128), [], [T("fbc")])

    blocks = [ob0 - 1] + list(range(ob0, ob0 + nblk))
    sts = []
    i = 0
    while i < len(blocks):
        sts.append(blocks[i:i + 9])
        i += 9
    NTMAX = 9 * 128
    hT2 = [b.sb("hT", [128, 8, NTMAX], BF16) for _ in range(2)]
    g = b.sb("g", [128, 22, NTMAX], BF16)
    ahalo = b.sb("ahalo", [128, 44, 2], BF16)
    b.memset(ahalo, 0.0, [T("ahalo")])
    xt = [b.sb("xt", [128, D], F32) for _ in range(2)]
    hb = [b.sb("hb", [128, D], BF16) for _ in range(2)]
    st = [b.sb("st", [128, 4], F32) for _ in range(2)]
    junk = b.sb("junk", [128, D], F32)
    wbuf = [b.sb("wbuf", [128, 8, 512], BF16) for _ in range(3)]
    a_sb = [b.sb("a_sb", [128, 2 + 512], BF16) for _ in range(2)]
    sg = b.sb("sg", [128, 9 * 128], F32)
    dg = [[b.sb("dg", [128, 3, 128], BF16) for _ in range(2)] for _ in range(2)]
    xo = [b.sb("xo", [128, D], F32) for _ in range(2)]
    ho = b.sb("ho", [128, D], F32)
    halo_flag = C["halo"]
    nw = 0
    def norm_block(sti, bi, ob):
        hT = hT2[sti % 2]
        s = bi % 2
        b.dma("sp", xt[s], src[ob * 128:(ob + 1) * 128, :], ["%s_%d" % (sk, ob)], [T("xt%d" % s)])
        b.rmsnorm(xt[s], T("xt%d" % s), gbc, T("gbc"), hb[s], T("hb%d" % s), st[s], T("st%d" % s), junk, T("junk"))
        pt = PB[:, 0:512].bitcast(BF16).rearrange("p (k t) -> p k t", t=128)
        for k in range(8):
            b.tr(pt[:, k, :], hb[s][:, k * 128:(k + 1) * 128], ident, [T("hb%d" % s)], ["P2"])
        if sti == 0 and bi == 0:
            b.ts(hT[:, :, bi * 128:(bi + 1) * 128], pt, halo_flag[:, 0:1], None, ALU.mult, None, ["P2", "halo"], [T("hT%d" % (sti % 2))])
        else:
            b.cpa(hT[:, :, bi * 128:(bi + 1) * 128], pt, ["P2"], [T("hT%d" % (sti % 2))])

    for bi, ob in enumerate(sts[0]):
        norm_block(0, bi, ob)
    for sti, blks in enumerate(sts):
        NT = 128 * len(blks)
        hT = hT2[sti % 2]
        hTk = T("hT%d" % (sti % 2))
        nxt = list(enumerate(sts[sti + 1])) if sti + 1 < len(sts) else []
        pieces = []
        c = 0
        while c < NT:
            pieces.append((c, min(512, NT - c)))
            c += 512
        pend = [None]
        uc = [0]

        def unit_U(ws, cc, half, ch, c0, n, u):
            pa = (PA[:, 0:512], "P0") if u % 2 == 0 else (PA[:, 512:1024], "P1")
            for k in range(8):
                b.mm(pa[0][:, 0:n], wbuf[ws][:, k, half * 256 + cc * 128: half * 256 + (cc + 1) * 128],
                     hT[:, k, c0:c0 + n], k == 0, k == 7, [T("wbuf%d" % ws), hTk], [pa[1]])
            asb = a_sb[u % 2]
            ak = T("asb%d" % (u % 2))
            b.cpv(asb[:, 0:2], ahalo[:, ch, :], [T("ahalo")], [ak])
            b.cpa(asb[:, 2:2 + n], pa[0][:, 0:n], [pa[1]], [ak])
            b.cpv(ahalo[:, ch, :], asb[:, n:n + 2], [ak], [T("ahalo")])

        def unit_V(fch, half, ch, c0, n, u, dgt, dgk):
            pc = (PC[:, 0:512], "P4") if u % 2 == 0 else (PC[:, 512:1024], "P5")
            asb = a_sb[u % 2]
            ak = T("asb%d" % (u % 2))
            for tap in range(3):
                b.mm(pc[0][:, 0:n], dgt[:, tap, :], asb[:, tap:tap + n], tap == 0, tap == 2, [dgk, ak], [pc[1]])
            if half == 0:
                b.act(sg[:, c0:c0 + n], pc[0][:, 0:n], AF.Silu, [pc[1], T("cwb")], [T("sg")], bias=cwb[:, 3, ch:ch + 1])
            else:
                b.stt(g[:, fch, c0:c0 + n], pc[0][:, 0:n], cwb[:, 3, ch:ch + 1], sg[:, c0:c0 + n], ALU.add, ALU.mult,
                      [pc[1], T("cwb"), T("sg")], [T("g")])

        for jj in range(11):
            if jj < len(nxt):
                norm_block(sti + 1, nxt[jj][0], nxt[jj][1])
            ws = nw % 3
            nw += 1
            wv = w_up.rearrange("(k p) n -> p k n", p=128)
            b.dma("pool", wbuf[ws][:, :, 0:256], wv[:, :, jj * 256:(jj + 1) * 256], [], [T("wbuf%d" % ws)])
            b.dma("pool", wbuf[ws][:, :, 256:512], wv[:, :, DFF + jj * 256:DFF + (jj + 1) * 256], [], [T("wbuf%d" % ws)])
            for cc in range(2):
                fch = jj * 2 + cc
                par = fch % 2
                for half in range(2):
                    ch = fch + 22 * half
                    for tap in range(3):
                        b.ts(dg[par][half][:, tap, :], ident, cwb[:, tap, ch:ch + 1], None, ALU.mult, None,
                             [T("cwb"), "ident"], [T("dg%d_%d" % (par, half))])
                for (c0, n) in pieces:
                    for half in range(2):
                        ch = fch + 22 * half
                        u = uc[0]
                        uc[0] += 1
                        unit_U(ws, cc, half, ch, c0, n, u)
                        if pend[0] is not None:
                            unit_V(*pend[0])
                        pend[0] = (fch, half, ch, c0, n, u, dg[par][half], T("dg%d_%d" % (par, half)))
        if pend[0] is not None:
            unit_V(*pend[0])
            pend[0] = None
        for bi, ob in enumerate(blks):
            if sti == 0 and bi == 0:
                continue
            s = bi % 2
            b.dma("sp", xt[s], src[ob * 128:(ob + 1) * 128, :], ["%s_%d" % (sk, ob)], [T("xt%d" % s)])
            for half in range(2):
                for j in range(22):
                    b.mm(PD[:, half * 512:(half + 1) * 512], g[:, j, bi * 128:(bi + 1) * 128], wdn[:, j, half * 512:(half + 1) * 512],
                         j == 0, j == 21, [T("g"), T("wdn")], ["P6" if half == 0 else "P7"])
            b.tt(xo[s], PD, xt[s], ALU.add, ["P6", "P7", T("xt%d" % s)], [T("xo%d" % s)])
            if not final:
                b.dma("sp", dst[ob * 128:(ob + 1) * 128, :], xo[s], [T("xo%d" % s)], ["%s_%d" % (dk, ob)])
            else:
                b.act(junk, xo[s], AF.Square, [T("xo%d" % s)], [T("junk"), T("st%d" % s)], accum_out=st[s][:, 0:1])
                b.ts(st[s][:, 1:2], st[s][:, 0:1], 1.0 / D, 1e-6, ALU.mult, ALU.add, [T("st%d" % s)], [T("st%d" % s)])
                b.act(st[s][:, 2:3], st[s][:, 1:2], AF.Sqrt, [T("st%d" % s)], [T("st%d" % s)])
                b.recip(st[s][:, 3:4], st[s][:, 2:3], [T("st%d" % s)], [T("st%d" % s)])
                b.stt(ho, xo[s], st[s][:, 3:4], fbc, ALU.mult, ALU.mult, [T("xo%d" % s), T("st%d" % s), T("fbc")], [T("ho")])
                b.dma("sp", dst[(ob - 2) * 128:(ob - 1) * 128, :], ho, [T("ho")], ["%s_%d" % (dk, ob)])


def stage_sgu(b, C, src, dst, sk, dk, tag="sgu"):
    T = lambda s: "%s_%s" % (tag, s)
    ident, identf = C["ident"], C["identf"]
    PA, PB, PC, PD = C["PA"], C["PB"], C["PC"], C["PD"]
    wuv = b.sb("wuv", [128, 8, 2048], BF16)
    load_w(b, "pool", wuv, C["sgu_w_uv"], 0, 2048, 0, T("wuv"))
    wout = b.sb("wout", [128, 8, D], BF16)
    load_w(b, "pool", wout, C["sgu_w_out"], 0, D, 0, T("wout"))
    gbc = b.sb("gbc", [128, D], F32)
    b.dma("sp", gbc, C["sgu_norm"].partition_broadcast(128), [], [T("gbc")])
    lng = b.sb("lng", [128, D], F32)
    b.dma("sp", lng, C["sgu_ln_g"].partition_broadcast(128), [], [T("lng")])
    lnb = b.sb("lnb", [128, D], F32)
    b.dma("sp", lnb, C["sgu_ln_b"].partition_broadcast(128), [], [T("lnb")])
    wsr = b.sb("wsr", [128, 8, 128], F32)
    b.dma("sp", wsr, C["sgu_w_s"].rearrange("g t s -> t g s"), [], [T("wsr")])
    wsm = b.sb("wsm", [128, 8, 128], BF16)
    b.tt(wsm, wsr, C["tril"].unsqueeze(1).to_broadcast([128, 8, 128]), ALU.mult, [T("wsr"), "tril"], [T("wsm")])
    wsT = b.sb("wsT", [128, 8, 128], BF16)
    pt = PB[:, 0:512].bitcast(BF16).rearrange("p (k t) -> p k t", t=128)
    for gi in range(8):
        b.tr(pt[:, gi, :], wsm[:, gi, :], ident, [T("wsm")], ["P2"])
    b.cpv(wsT, pt, ["P2"], [T("wsT")])
    bsr = b.sb("bsr", [8, 128], F32)
    b.dma("sp", bsr, C["sgu_b_s"], [], [T("bsr")])
    bsT = b.sb("bsT", [128, 8], F32)
    b.tr(PB[:, 512:520], bsr, identf[0:8, 0:8], [T("bsr")], ["P3"])
    b.cpv(bsT, PB[:, 512:520], ["P3"], [T("bsT")])

    xt = [b.sb("xt", [128, D], F32) for _ in range(3)]
    hb = b.sb("hb", [128, D], BF16)
    st = b.sb("st", [128, 8], F32)
    junk = b.sb("junk", [128, D], F32)
    hT = b.sb("hT", [128, 8, 128], BF16)
    u2 = [b.sb("u", [128, D], F32) for _ in range(2)]
    v = b.sb("v", [128, D], F32)
    v1 = b.sb("v1", [128, D], F32)
    vn2 = [b.sb("vn", [128, D], BF16) for _ in range(2)]
    gt = b.sb("gt", [128, D], BF16)
    gtT = b.sb("gtT", [128, 8, 128], BF16)
    xo = [b.sb("xo", [128, D], F32) for _ in range(2)]

    def p1(ob):
        s = ob % 2
        sx = ob % 3
        u, vn = u2[s], vn2[s]
        uk, vnk = T("u%d" % s), T("vn%d" % s)
        b.dma("sp", xt[sx], src[ob * 128:(ob + 1) * 128, :], ["%s_%d" % (sk, ob)], [T("xt%d" % sx)])
        b.rmsnorm(xt[sx], T("xt%d" % sx), gbc, T("gbc"), hb, T("hb"), st, T("st"), junk, T("junk"))
        for k in range(8):
            b.tr(pt[:, k, :], hb[:, k * 128:(k + 1) * 128], ident, [T("hb")], ["P2"])
        b.cpa(hT, pt, ["P2"], [T("hT")])
        zb = [(PA[:, 0:512], "P0"), (PA[:, 512:1024], "P1"), (PC[:, 0:512], "P4"), (PC[:, 512:1024], "P5")]
        for gi in range(4):
            for k in range(8):
                b.mm(zb[gi][0], hT[:, k, :], wuv[:, k, gi * 512:(gi + 1) * 512], k == 0, k == 7, [T("hT"), T("wuv")], [zb[gi][1]])
        b.act(u[:, 0:512], zb[0][0], AF.Gelu_apprx_tanh, ["P0"], [uk])
        b.act(u[:, 512:1024], zb[1][0], AF.Gelu_apprx_tanh, ["P1"], [uk])
        b.act(v[:, 0:512], zb[2][0], AF.Gelu_apprx_tanh, ["P4"], [T("v"), T("st")], accum_out=st[:, 4:5])
        b.act(v[:, 512:1024], zb[3][0], AF.Gelu_apprx_tanh, ["P5"], [T("v"), T("st")], accum_out=st[:, 5:6])
        b.act(junk, v, AF.Square, [T("v")], [T("junk"), T("st")], accum_out=st[:, 6:7])
        b.tt(st[:, 4:5], st[:, 4:5], st[:, 5:6], ALU.add, [T("st")], [T("st")])
        b.ts(st[:, 4:5], st[:, 4:5], 1.0 / D, None, ALU.mult, None, [T("st")], [T("st")])
        b.tt(st[:, 5:6], st[:, 4:5], st[:, 4:5], ALU.mult, [T("st")], [T("st")])
        b.stt(st[:, 6:7], st[:, 6:7], 1.0 / D, st[:, 5:6], ALU.mult, ALU.subtract, [T("st")], [T("st")])
        b.ts(st[:, 6:7], st[:, 6:7], 1e-5, None, ALU.add, None, [T("st")], [T("st")])
        b.act(st[:, 7:8], st[:, 6:7], AF.Sqrt, [T("st")], [T("st")])
        b.recip(st[:, 7:8], st[:, 7:8], [T("st")], [T("st")])
        b.stt(v1, v, st[:, 4:5], lng, ALU.subtract, ALU.mult, [T("v"), T("st"), T("lng")], [T("v1")])
        b.stt(vn, v1, st[:, 7:8], lnb, ALU.mult, ALU.add, [T("v1"), T("st"), T("lnb")], [vnk])

    def p2(ob):
        s = ob % 2
        sx = ob % 3
        u, vn = u2[s], vn2[s]
        uk, vnk = T("u%d" % s), T("vn%d" % s)
        for gi in range(8):
            b.mm(PD[:, gi * 128:(gi + 1) * 128], wsT[:, gi, :], vn[:, gi * 128:(gi + 1) * 128], True, True,
                 [T("wsT"), vnk], ["P6" if gi < 4 else "P7"])
        for gi in range(8):
            b.stt(gt[:, gi * 128:(gi + 1) * 128], PD[:, gi * 128:(gi + 1) * 128], bsT[:, gi:gi + 1], u[:, gi * 128:(gi + 1) * 128],
                  ALU.add, ALU.mult, ["P6" if gi < 4 else "P7", T("bsT"), uk], [T("gt")])
        for k in range(8):
            b.tr(pt[:, k, :], gt[:, k * 128:(k + 1) * 128], ident, [T("gt")], ["P2"])
        b.cpa(gtT, pt, ["P2"], [T("gtT")])
        for half in range(2):
            for k in range(8):
                b.mm(PA[:, half * 512:(half + 1) * 512], gtT[:, k, :], wout[:, k, half * 512:(half + 1) * 512], k == 0, k == 7,
                     [T("gtT"), T("wout")], ["P0" if half == 0 else "P1"])
        b.tt(xo[s], PA, xt[sx], ALU.add, ["P0", "P1", T("xt%d" % sx)], [T("xo%d" % s)])
        b.dma("sp", dst[ob * 128:(ob + 1) * 128, :], xo[s], [T("xo%d" % s)], ["%s_%d" % (dk, ob)])

    p1(1)
    for ob in range(1, NOWN):
        if ob + 1 < NOWN:
            p1(ob + 1)
        p2(ob)


def stage_l0(b, C, xloc, dst, dk, tag="l0", first_own=OWN0, nblocks=NB):
    T = lambda s: "%s_%s" % (tag, s)
    ident, identf = C["ident"], C["identf"]
    PA, PB, PC, PD = C["PA"], C["PB"], C["PC"], C["PD"]
    w_in = C["attn_w_in"]
    wk = b.sb("wk", [128, 8, 468], BF16)
    for (c0, c1, d0) in ((256, 512, 0), (1536, 1552, 256), (2064, 2128, 272), (2448, 2512, 336), (2128, 2192, 400), (2512, 2516, 464)):
        load_w(b, "pool", wk, w_in, c0, c1, d0, T("wk"))
    wv = b.sb("wv", [128, 8, 512], BF16)
    load_w(b, "pool", wv, w_in, 512, 1024, 0, T("wv"))
    wq = b.sb("wq", [128, 8, 1536], BF16)
    for (c0, c1, d0) in ((0, 256, 0), (2192, 2448, 256), (1552, 2064, 512), (1024, 1536, 1024)):
        load_w(b, "pool", wq, w_in, c0, c1, d0, T("wq"))
    wo = b.sb("wo", [128, 8, D], BF16)
    load_w(b, "pool", wo, C["attn_w_o"], 0, D, 0, T("wo"))
    PCUT = int(os.environ.get("L0_PCUT", "99"))
    if PCUT <= 0:
        return
    wa2 = b.sb("wa2", [16, 256], BF16)
    b.dma("pool", wa2, C["gla_w_a2"], [], [T("wa2")])
    ba = b.sb("ba", [1, 256], BF16)
    b.dma("pool", ba, C["gla_b_a"], [], [T("ba")])
    hg = b.sb("hg", [128, 128], F32)
    b.dma("sp", hg, C["gla_head_g"].partition_broadcast(128), [], [T("hg")])
    gbc = b.sb("gbc", [128, D], F32)
    b.dma("sp", gbc, C["attn_norm"].partition_broadcast(128), [], [T("gbc")])
    ones1 = b.sb("ones1", [1, 128], BF16)
    b.memset(ones1, 1.0, [T("ones1")])
    kbias = b.sb("kbias", [128, NB], F32)
    b.dma("sp", kbias, C["kbias"], [], [T("kbias")])
    cb = b.sb("cb", [128, 128], BF16)
    b.dma("pool", cb, C["cbias"], [], [T("cb")])
    i4 = b.sb("i4", [128, 4, 128], BF16)
    for r4 in range(4):
        b.cpv(i4[:, r4, :], ident, ["ident"], [T("i4")])
    mUT = b.sb("mUT", [128, 128], F32)
    b.dma("sp", mUT, C["triu"], [], [T("mUT")])
    gU = b.sb("gU", [128, 128], BF16)
    b.dma("pool", gU, C["gU"], [], [T("gU")])
    gM1 = b.sb("gM1", [128, 128], BF16)
    b.dma("pool", gM1, C["gM1"], [], [T("gM1")])
    gLs = b.sb("gLs", [128, 128], BF16)
    b.dma("pool", gLs, C["gLs"], [], [T("gLs")])
    pw = b.sb("pw", [128, NBIS + 1], F32)
    b.dma("sp", pw, C["pw"].partition_broadcast(128), [], [T("pw")])
    if PCUT <= 1:
        return
    pos = b.sb("pos", [128, NB], F32)
    b.dma("sp", pos, C["pos"], [], [T("pos")])
    invf = b.sb("invf", [128, 8], F32)
    b.dma("sp", invf, C["invf"].partition_broadcast(128), [], [T("invf")])
    ang = b.sb("ang", [128, NB, 8], F32)
    rr = b.sb("rr", [128, NB, 8], F32)
    ki = b.sb("ki", [128, NB, 8], I32)
    kf = b.sb("kf", [128, NB, 8], F32)
    mfix = b.sb("mfix", [128, NB, 8], F32)
    cosT = b.sb("cosT", [128, NB, 8], F32)
    sinT = b.sb("sinT", [128, NB, 8], F32)
    TWO_PI = float(2 * np.pi)
    b.tt(ang, pos.unsqueeze(2).to_broadcast([128, NB, 8]), invf.unsqueeze(1).to_broadcast([128, NB, 8]), ALU.mult,
         [T("pos"), T("invf")], [T("ang")])
    b.ts(rr, ang, 1.0 / TWO_PI, None, ALU.mult, None, [T("ang")], [T("rr")])
    b.cpv(ki, rr, [T("rr")], [T("ki")])
    b.cpv(kf, ki, [T("ki")], [T("kf")])
    b.stt(rr, kf, -TWO_PI, ang, ALU.mult, ALU.add, [T("kf"), T("ang")], [T("rr")])

    def wrap(t, key):
        b.ts(mfix, t, float(np.pi), -TWO_PI, ALU.is_gt, ALU.mult, [key], [T("mfix")])
        b.tt(t, t, mfix, ALU.add, [key, T("mfix")], [key])
        b.ts(mfix, t, float(-np.pi), TWO_PI, ALU.is_lt, ALU.mult, [key], [T("mfix")])
        b.tt(t, t, mfix, ALU.add, [key, T("mfix")], [key])

    wrap(rr, T("rr"))
    b.act(sinT, rr, AF.Sin, [T("rr")], [T("sinT")])
    b.ts(rr, rr, float(np.pi / 2), None, ALU.add, None, [T("rr")], [T("rr")])
    wrap(rr, T("rr"))
    b.act(cosT, rr, AF.Sin, [T("rr")], [T("cosT")])

    if PCUT <= 2:
        return
    kkT = b.sb("kkT", [128, 2, NB * 128], BF16)
    bva = b.sb("bva", [128, NB, 66], BF16)
    b.memset(bva, 1.0, [T("bva")])
    if PCUT <= 3:
        return
    Sf = b.sb("Sf", [128, 2, 128], F32)
    b.memset(Sf, 0.0, [T("Sf")])
    if PCUT <= 4:
        return
    Sb = b.sb("Sb", [128, 2, 128], BF16)
    b.memset(Sb, 0.0, [T("Sb")])

    xt = [b.sb("xt", [128, D], F32) for _ in range(3)]
    hb = b.sb("hb", [128, D], BF16)
    st = b.sb("st", [128, 4], F32)
    junk = b.sb("junk", [128, D], F32)
    hT = b.sb("hT", [128, 8, 128], BF16)
    k1s = b.sb("k1s", [128, 468], F32)
    vb = b.sb("vb", [128, 512], BF16)
    q1s = b.sb("q1s", [128, 512], F32)
    q2s = b.sb("q2s", [128, 512], F32)
    sar = b.sb("sar", [128, 512], F32)
    kk = b.sb("kk", [128, 256], BF16)
    ropA = b.sb("ropA", [128, 8, 2, 8], F32)
    ropB = b.sb("ropB", [128, 8, 2, 8], F32)
    alrb = b.sb("alrb", [128, 16], BF16)
    alrT = b.sb("alrT", [16, 128], BF16)
    e1 = b.sb("e1", [128, 256], F32)
    l1 = b.sb("l1", [128, 256], F32)
    l1h = b.sb("l1h", [128, 256], BF16)
    l1r = b.sb("l1r", [128, 256], F32)
    l1l = b.sb("l1l", [128, 256], BF16)
    E0 = b.sb("E0", [128, 2, 128], F32)
    E1 = b.sb("E1", [128, 2, 128], F32)
    E2 = b.sb("E2", [128, 2, 128], F32)
    E3 = b.sb("E3", [128, 256], F32)
    khat = b.sb("khat", [128, 256], BF16)
    qkb = b.sb("qkb", [128, 512], BF16)
    qtl = b.sb("qtl", [128, 2, 128], BF16)
    ktlz = [b.sb("ktlz", [128, 2, 128], BF16) for _ in range(2)]
    qhz = [b.sb("qhz", [128, 2, 128], BF16) for _ in range(2)]
    rmk = C["rowmask"]
    attm = b.sb("attm", [128, 4, 128], BF16)
    osb = b.sb("osb", [128, 4, 128], F32)
    osq = b.sb("osq", [128, 4, 128], F32)
    gst = b.sb("gst", [128, 12], F32)
    Gg = b.sb("Gg", [128, 4, 128], F32)
    cat2 = [b.sb("cat", [128, D], BF16) for _ in range(2)]
    catT = b.sb("catT", [128, 8, 128], BF16)
    iqs = b.sb("iqs", [128, 256], F32)
    iqb = b.sb("iqb", [128, 256], BF16)
    bqb = b.sb("bqb", [128, 512], BF16)
    iqz = b.sb("iqz", [128, 2, 2, 128], BF16)
    bqz2 = [[b.sb("bqz", [128, 4, 128], BF16) for _ in range(2)] for _ in range(2)]
    wab = b.sb("wab", [128, 8], F32)
    sgn = b.sb("sgn", [128, 4], F32)
    sd = b.sb("sd", [128, 4, 128], BF16)
    Rr = [b.sb("Rr", [128, 512], BF16) for _ in range(4)]
    scr = b.sb("scr", [128, NB * 128], F32)
    mb2 = [b.sb("mb", [128, NB * 128], BF16) for _ in range(2)]
    bis = b.sb("bis", [128, 8], F32)
    stp = b.sb("stp", [128, NBIS + 1], F32)
    stp2 = b.sb("stp2", [128, NBIS + 1], F32)
    mid = [b.sb("mid", [128, 1], F32) for _ in range(2)]
    cnt = b.sb("cnt", [128, 1], F32)
    dd = b.sb("dd", [128, 1], F32)
    PT = [b.sb("PT", [128, 1024], BF16) for _ in range(2)]
    den = b.sb("den", [128, 8], F32)

    ptb = PB[:, 0:512].bitcast(BF16).rearrange("p (k t) -> p k t", t=128)

    def rope(srcv, dstv, H, blk, rk, wk_):
        cb_ = cosT[:, blk, :].unsqueeze(1).to_broadcast([128, H, 8])
        sb_ = sinT[:, blk, :].unsqueeze(1).to_broadcast([128, H, 8])
        x1 = srcv[:, :, 0:8]
        x2 = srcv[:, :, 8:16]
        A = ropA[:, 0:H]
        Bm = ropB[:, 0:H]
        b.tt(A[:, :, 0, :], x1, cb_, ALU.mult, rk + [T("cosT")], [T("ropA")])
        b.tt(A[:, :, 1, :], x2, cb_, ALU.mult, rk + [T("cosT")], [T("ropA")])
        b.tt(Bm[:, :, 0, :], x1, sb_, ALU.mult, rk + [T("sinT")], [T("ropB")])
        b.tt(Bm[:, :, 1, :], x2, sb_, ALU.mult, rk + [T("sinT")], [T("ropB")])
        b.tt(dstv[:, :, 0:8], A[:, :, 0, :], Bm[:, :, 1, :], ALU.subtract, [T("ropA"), T("ropB")], wk_)
        b.tt(dstv[:, :, 8:16], A[:, :, 1, :], Bm[:, :, 0, :], ALU.add, [T("ropA"), T("ropB")], wk_)
        b.cpa(dstv[:, :, 16:64], srcv[:, :, 16:64], rk, wk_)

    def part_A(blk):
        own = blk >= first_own
        s = blk % 2
        sx = blk % 3
        xk = T("xt%d" % sx)
        cat = cat2[s]
        bqz = bqz2[s]
        b.dma("sp", xt[sx], xloc[blk * 128:(blk + 1) * 128, :], [], [xk])
        b.rmsnorm(xt[sx], xk, gbc, T("gbc"), hb, T("hb"), st, T("st"), junk, T("junk"))
        for k in range(8):
            b.tr(ptb[:, k, :], hb[:, k * 128:(k + 1) * 128], ident, [T("hb")], ["P2"])
        b.cpa(hT, ptb, ["P2"], [T("hT")])
        for k in range(8):
            b.mm(PA[:, 0:468], hT[:, k, :], wk[:, k, :], k == 0, k == 7, [T("hT"), T("wk")], ["P0"])
        b.cpv(k1s, PA[:, 0:468], ["P0"], [T("k1s")])
        for k in range(8):
            b.mm(PA[:, 512:1024], hT[:, k, :], wv[:, k, :], k == 0, k == 7, [T("hT"), T("wv")], ["P1"])
        b.cpa(vb, PA[:, 512:1024], ["P1"], [T("vb")])
        if own:
            for k in range(8):
                b.mm(PA[:, 0:512], hT[:, k, :], wq[:, k, 0:512], k == 0, k == 7, [T("hT"), T("wq")], ["P0"])
            b.cpv(q1s, PA[:, 0:512], ["P0"], [T("q1s")])
            for k in range(8):
                b.mm(PA[:, 512:1024], hT[:, k, :], wq[:, k, 512:1024], k == 0, k == 7, [T("hT"), T("wq")], ["P1"])
            b.cpa(q2s, PA[:, 512:1024], ["P1"], [T("q2s")])
            for k in range(8):
                b.mm(PA[:, 0:512], hT[:, k, :], wq[:, k, 1024:1536], k == 0, k == 7, [T("hT"), T("wq")], ["P0"])
            b.act(sar, PA[:, 0:512], AF.Silu, ["P0"], [T("sar")])
        kkv = kk[:, 0:128].rearrange("p (h e) -> p h e", e=64)
        rope(k1s[:, 272:400].rearrange("p (h e) -> p h e", e=64), kkv, 2, blk, [T("k1s")], [T("kk")])
        b.cpv(kk[:, 128:192], kk[:, 64:128], [T("kk")], [T("kk2")])
        b.cpv(kk[:, 192:256], kk[:, 0:64], [T("kk")], [T("kk2")])
        b.tr(ptb[:, 0, :], kk[:, 0:128], ident, [T("kk")], ["P2"])
        b.tr(ptb[:, 1, :], kk[:, 128:256], ident, [T("kk"), T("kk2")], ["P2"])
        cs = slice(blk * 128, (blk + 1) * 128)
        bkk, ikk = T("bkT_%d" % blk), T("ikT_%d" % blk)
        b.cpv(kkT[:, :, cs], ptb[:, 0:2, :], ["P2"], [bkk, ikk])
        b.cpv(bva[:, blk, 0:64], k1s[:, 400:464], [T("k1s")], [T("bva_%d" % blk)])
        b.cpv(alrb, k1s[:, 256:272], [T("k1s")], [T("alrb")])
        b.tr(ptb[0:16, 2, :], alrb, ident, [T("alrb")], ["P2"])
        b.cpv(alrT, ptb[0:16, 2, :], ["P2"], [T("alrT")])
        zP = PB[:, 512:768]
        b.mm(zP, alrT, wa2, True, False, [T("alrT"), T("wa2")], ["P3"])
        b.mm(zP, ones1, ba, False, True, [T("ones1"), T("ba")], ["P3"])
        b.act(e1, zP, AF.Exp, ["P3"], [T("e1")], scale=-1.0)
        b.act(l1, e1, AF.Ln, [T("e1")], [T("l1")], bias=1.0)
        b.cpv(l1h, l1, [T("l1")], [T("l1h")])
        b.tt(l1r, l1, l1h, ALU.subtract, [T("l1"), T("l1h")], [T("l1r")])
        b.cpv(l1l, l1r, [T("l1r")], [T("l1l")])
        X3 = PB[:, 768:1024]
        b.mm(X3, gLs, l1h, True, False, [T("gLs"), T("l1h")], ["P3"])
        b.mm(X3, gLs, l1l, False, True, [T("gLs"), T("l1l")], ["P3"])
        b.act(E3, X3, AF.Exp, ["P3"], [T("E3")])
        b.tt(khat, k1s[:, 0:256], E3, ALU.mult, [T("k1s"), T("E3")], [T("khat")])
        X0 = PC[:, 0:256].rearrange("p (a t) -> p a t", t=128)
        X1 = PC[:, 256:512].rearrange("p (a t) -> p a t", t=128)
        for hp in range(2):
            b.mm(X0[:, hp, :], l1h[:, hp * 128:(hp + 1) * 128], gU, True, False, [T("l1h"), T("gU")], ["P4"])
            b.mm(X0[:, hp, :], l1l[:, hp * 128:(hp + 1) * 128], gU, False, True, [T("l1l"), T("gU")], ["P4"])
        b.act(E0, X0, AF.Exp, ["P4"], [T("E0")])
        if own:
            for hp in range(2):
                b.mm(X1[:, hp, :], l1h[:, hp * 128:(hp + 1) * 128], gM1, True, False, [T("l1h"), T("gM1")], ["P4"])
                b.mm(X1[:, hp, :], l1l[:, hp * 128:(hp + 1) * 128], gM1, False, True, [T("l1l"), T("gM1")], ["P4"])
            b.act(E1, X1, AF.Exp, ["P4"], [T("E1")])
            b.act(E2, X1, AF.Exp, ["P4"], [T("E2")], scale=-1.0)
            b.cpv(qkb[:, 0:256], q1s[:, 0:256], [T("q1s")], [T("qkb")])
            b.cpa(qkb[:, 256:512], k1s[:, 0:256], [T("k1s")], [T("qkb")])
            for c4 in range(4):
                b.tr(ptb[:, 4 + c4, :], qkb[:, c4 * 128:(c4 + 1) * 128], ident, [T("qkb")], ["P2"])
            b.stt(qtl, ptb[:, 4:6, :], 0.125, E1, ALU.mult, ALU.mult, ["P2", T("E1")], [T("qtl")])
            for h2_ in range(2):
                b.stt(qhz[h2_], ptb[:, 4:6, :], rmk[:, 2 + h2_:3 + h2_], E0, ALU.mult, ALU.mult, ["P2", T("E0"), "rowmask"], [T("qhat")])
                b.stt(ktlz[h2_], ptb[:, 6:8, :], rmk[:, h2_:h2_ + 1], E2, ALU.mult, ALU.mult, ["P2", T("E2"), "rowmask"], [T("ktl")])
            aT = PC[:, 512:1024].rearrange("p (h t) -> p h t", t=128)
            for h in range(4):
                hp, h2 = h // 2, h % 2
                rs = slice(h2 * 64, (h2 + 1) * 64)
                b.mm(aT[:, h, :], ktlz[h2][:, hp, :], qtl[:, hp, :], True, True, [T("ktl"), T("qtl")], ["P5"])
            b.tt(attm, aT, mUT.unsqueeze(1).to_broadcast([128, 4, 128]), ALU.mult, ["P5", T("mUT")], [T("attm")])
            oP = PD[:, 0:512].rearrange("p (h t) -> p h t", t=128)
            for h in range(4):
                hp, h2 = h // 2, h % 2
                rs = slice(h2 * 64, (h2 + 1) * 64)
                b.mm(oP[:, h, :], attm[:, h, :], vb[:, h * 128:(h + 1) * 128], True, False, [T("attm"), T("vb")], ["P6"])
                b.mm(oP[:, h, :], qhz[h2][:, hp, :], Sb[:, hp, :], False, True, [T("qhat"), T("Sb")], ["P6"])
            b.cpa(osb, oP, ["P6"], [T("osb")])
            b.tt(osq, osb, osb, ALU.mult, [T("osb")], [T("osq")])
            b.S.dve(lambda e: e.tensor_reduce(out=gst[:, 0:4], in_=osq, axis=AX.X, op=ALU.add), [T("osq")], [T("gst")])
            b.ts(gst[:, 4:8], gst[:, 0:4], 1.0 / 128, 1e-6, ALU.mult, ALU.add, [T("gst")], [T("gst")])
            b.act(gst[:, 8:12], gst[:, 4:8], AF.Sqrt, [T("gst")], [T("gst2")])
            b.recip(gst[:, 4:8], gst[:, 8:12], [T("gst2")], [T("gst")])
            b.tt(Gg, sar.rearrange("p (h t) -> p h t", t=128), hg.unsqueeze(1).to_broadcast([128, 4, 128]), ALU.mult,
                 [T("sar"), T("hg")], [T("Gg")])
            b.tt(osq, osb, gst[:, 4:8].unsqueeze(2).to_broadcast([128, 4, 128]), ALU.mult, [T("osb"), T("gst")], [T("osq")])
            b.tt(cat[:, 0:512].rearrange("p (h t) -> p h t", t=128), osq, Gg, ALU.mult, [T("osq"), T("Gg")], [T("cat%d" % (blk % 2))])
        dS = PD[:, 512:1024].rearrange("p (a t) -> p a t", t=256)
        for hp in range(2):
            b.mm(dS[:, hp, :], khat[:, hp * 128:(hp + 1) * 128], vb[:, hp * 256:(hp + 1) * 256], True, True,
                 [T("khat"), T("vb")], ["P7"])
        dSs = osq.rearrange("p h t -> p (h t)").rearrange("p (a t) -> p a t", t=256)
        b.cpa(dSs, dS, ["P7"], [T("osq")])
        for hp in range(2):
            for h2 in range(2):
                rs = slice(h2 * 64, (h2 + 1) * 64)
                b.stt(Sf[rs, hp, :], Sf[rs, hp, :], E0[rs, hp, 127:128], dSs[rs, hp, h2 * 128:(h2 + 1) * 128], ALU.mult, ALU.add,
                      [T("Sf"), T("E0"), T("osq")], [T("Sf")])
        b.cpa(Sb, Sf, [T("Sf")], [T("Sb")])
        if not own:
            return
        nk = blk + 1
        NK = nk * 128
        b.act(wab[:, 0:4], k1s[:, 464:468], AF.Abs, [T("k1s")], [T("wab")], scale=0.0625)
        b.act(sgn, k1s[:, 464:468], AF.Sign, [T("k1s")], [T("sgn")])
        for h in range(4):
            b.ts(sd[:, h, :], ident, sgn[:, h:h + 1], None, ALU.mult, None, [T("sgn"), "ident"], [T("sd")])
        b.tt(iqs.rearrange("p (h e) -> p h e", e=64), q1s[:, 256:512].rearrange("p (h e) -> p h e", e=64),
             wab[:, 0:4].unsqueeze(2).to_broadcast([128, 4, 64]), ALU.mult, [T("q1s"), T("wab")], [T("iqs")])
        rope(iqs.rearrange("p (h e) -> p h e", e=64), iqb.rearrange("p (h e) -> p h e", e=64), 4, blk, [T("iqs")], [T("iqb")])
        rope(q2s.rearrange("p (h e) -> p h e", e=64), bqb.rearrange("p (h e) -> p h e", e=64), 8, blk, [T("q2s")], [T("bqb")])
        for c2 in range(2):
            b.tr(ptb[:, c2, :], iqb[:, c2 * 128:(c2 + 1) * 128], ident, [T("iqb")], ["P2"])
        for c4 in range(4):
            b.tr(ptb[:, 2 + c4, :], bqb[:, c4 * 128:(c4 + 1) * 128], ident, [T("bqb")], ["P2"])
        for h2_ in range(2):
            b.ts(iqz[:, h2_, :, :], ptb[:, 0:2, :], rmk[:, h2_:h2_ + 1], None, ALU.mult, None, ["P2", "rowmask"], [T("iqT")])
            b.ts(bqz[h2_], ptb[:, 2:6, :], rmk[:, h2_:h2_ + 1], None, ALU.mult, None, ["P2", "rowmask"], [T("bqT%d" % (blk % 2))])
        b.S.dve(lambda e: e.tensor_reduce(out=bis[:, 0:1], in_=wab[:, 0:4], axis=AX.X, op=ALU.add), [T("wab")], [T("bis")])
        b.ts(bis[:, 1:2], bis[:, 0:1], 64.0, 1e-3, ALU.mult, ALU.add, [T("bis")], [T("bis")])
        b.ts(stp, pw, bis[:, 1:2], None, ALU.mult, None, [T("pw"), T("bis")], [T("stp")])
        b.ts(stp2, stp, 2.0, None, ALU.mult, None, [T("stp")], [T("stp2")])
        ntile = (nk + 3) // 4
        for kt in range(ntile):
            nkb = min(4, nk - 4 * kt)
            N = 128 * nkb
            c0 = kt * 512
            ikkeys = [T("ikT_%d" % j) for j in range(kt * 4, kt * 4 + nkb)]
            for rnd in range(2):
                for h2 in range(2):
                    h = 2 * rnd + h2
                    rs = slice(h2 * 64, (h2 + 1) * 64)
                    pb = (PA[:, 0:512], "P0") if h2 == 0 else (PA[:, 512:1024], "P1")
                    b.mm(pb[0][:, 0:N], iqz[:, h2, rnd, :], kkT[:, 1 - h2, c0:c0 + N], True, True, [T("iqT")] + ikkeys, [pb[1]])
                    b.act(Rr[h][:, 0:N], pb[0][:, 0:N], AF.Relu, [pb[1]], [T("Rr%d" % h)])
            sP = PB[:, 512:1024]
            last_tile = kt == ntile - 1
            for h in range(4):
                b.mm(sP[:, 0:N], sd[:, h, :], Rr[h][:, 0:N], h == 0, (h == 3) and not last_tile, [T("sd"), T("Rr%d" % h)], ["P3"])
            if last_tile:
                off = (nkb - 1) * 128
                b.mm(sP[:, off:off + 128], ident, cb, False, True, [T("cb"), "ident"], ["P3"])
            b.tt(scr[:, c0:c0 + N].rearrange("p (a t) -> p a t", t=128), sP[:, 0:N].rearrange("p (a t) -> p a t", t=128),
                 kbias[:, kt * 4:kt * 4 + nkb].unsqueeze(2).to_broadcast([128, nkb, 128]), ALU.add, ["P3", T("kbias")], [T("scr")])
    def part_B(blk):
        nk = blk + 1
        NK = nk * 128
        mb = mb2[blk % 2]
        jb = mb
        b.memset(mid[0], 0.0, [T("mid0")])
        for it in range(NBIS):
            mi, mo = it % 2, (it + 1) % 2
            b.ts(jb[:, 0:NK], scr[:, 0:NK], mid[mi][:, 0:1], None, ALU.is_gt, ALU.add, [T("scr"), T("mid%d" % mi)],
                 [T("mb%d" % (blk % 2)), T("cnt")], accum_out=cnt[:, 0:1])
            b.ts(dd, cnt, 255.5, stp2[:, it:it + 1], ALU.is_gt, ALU.mult, [T("cnt"), T("stp2")], [T("dd")])
            b.stt(mid[mo], dd, stp[:, it:it + 1], mid[mi], ALU.subtract, ALU.add, [T("dd"), T("stp"), T("mid%d" % mi)],
                  [T("mid%d" % mo)])
        mf = NBIS % 2
        b.tt(bis[:, 2:3], mid[mf], stp[:, NBIS:NBIS + 1], ALU.subtract, [T("mid%d" % mf), T("stp")], [T("bis2")])
        b.ts(mb[:, 0:NK], scr[:, 0:NK], bis[:, 2:3], NEG, ALU.is_le, ALU.mult, [T("scr"), T("bis2")], [T("mb%d" % (blk % 2))])
    def part_C(blk):
        nk = blk + 1
        s = blk % 2
        sx = blk % 3
        xk = T("xt%d" % sx)
        mb = mb2[s]
        cat = cat2[s]
        bqz = bqz2[s]
        Ov = PA.rearrange("p (two x) -> p two x", two=2)[:, :, 0:260].rearrange("p two (h e) -> p two h e", e=65)
        bq0 = [bqz[0].rearrange("p c t -> p (c t)"), bqz[1].rearrange("p c t -> p (c t)")]
        for kb in range(nk):
            ks = slice(kb * 128, (kb + 1) * 128)
            STt = (PC, "P4", "P5") if kb % 2 == 0 else (PA, "P0", "P1")
            ST = STt[0]
            b.mm(ST[:, 0:512], kkT[:, 0, ks], bq0[0], True, False, [T("bkT_%d" % kb), T("bqT%d" % (blk % 2))], [STt[1]])
            b.mm(ST[:, 0:512], mb[:, ks], i4.rearrange("p c t -> p (c t)"), False, True, [T("mb%d" % (blk % 2)), T("i4")], [STt[1]])
            b.mm(ST[:, 512:1024], kkT[:, 1, ks], bq0[1], True, False, [T("bkT_%d" % kb), T("bqT%d" % (blk % 2))], [STt[2]])
            b.mm(ST[:, 512:1024], mb[:, ks], i4.rearrange("p c t -> p (c t)"), False, True, [T("mb%d" % (blk % 2)), T("i4")], [STt[2]])
            pt_ = PT[kb % 2]
            b.act(pt_, ST, AF.Exp, [STt[1], STt[2]], [T("PT%d" % (kb % 2))], scale=0.125)
            for half in range(2):
                b.mm(PD[0:65, half * 512:(half + 1) * 512], bva[:, kb, 0:65], pt_[:, half * 512:(half + 1) * 512], kb == 0, kb == nk - 1,
                     [T("PT%d" % (kb % 2)), T("bva_%d" % kb)], ["P6" if half == 0 else "P7"])
        OTs = junk[0:65, :]
        b.cpa(OTs, PD[0:65, :], ["P6", "P7"], [T("junk")])
        for hh in range(8):
            h2, c4 = hh // 4, hh % 4
            head = 2 * c4 + h2
            b.tr(Ov[:, head // 4, head % 4, :], OTs[:, hh * 128:(hh + 1) * 128], identf[0:65, 0:65], [T("junk")],
                 ["P0" if head < 4 else "P1"])
        for bank in range(2):
            pk = "P0" if bank == 0 else "P1"
            b.ts(den[:, bank * 4:(bank + 1) * 4], Ov[:, bank, :, 64], 1e-30, None, ALU.add, None, [pk], [T("den")])
        b.recip(den, den, [T("den")], [T("den")])
        for bank in range(2):
            pk = "P0" if bank == 0 else "P1"
            b.tt(cat[:, 512 + bank * 256:512 + (bank + 1) * 256].rearrange("p (h e) -> p h e", e=64), Ov[:, bank, :, 0:64],
                 den[:, bank * 4:(bank + 1) * 4].unsqueeze(2).to_broadcast([128, 4, 64]), ALU.mult, [pk, T("den")], [T("cat%d" % (blk % 2))])
        for k in range(8):
            b.tr(ptb[:, k, :], cat[:, k * 128:(k + 1) * 128], ident, [T("cat%d" % (blk % 2))], ["P2"])
        b.cpa(catT, ptb, ["P2"], [T("catT")])
        for half in range(2):
            for k in range(8):
                b.mm(PA[:, half * 512:(half + 1) * 512], catT[:, k, :], wo[:, k, half * 512:(half + 1) * 512], k == 0, k == 7,
                     [T("catT"), T("wo")], ["P0" if half == 0 else "P1"])
        b.tt(xt[sx], PA, xt[sx], ALU.add, ["P0", "P1", xk], [xk])
        ob = blk - first_own
        b.dma("sp", dst[ob * 128:(ob + 1) * 128, :], xt[sx], [xk], ["%s_%d" % (dk, ob)])


    prev = None
    for blk in range(nblocks):
        part_A(blk)
        if blk >= first_own:
            part_B(blk)
        if prev is not None:
            part_C(prev)
        prev = blk if blk >= first_own else None
    if prev is not None:
        part_C(prev)


WEIGHT_SPECS = [
    ("attn_norm", [1, D]), ("attn_w_in", [D, 2516]), ("gla_w_a2", [16, 256]), ("gla_b_a", [1, 256]),
    ("gla_head_g", [1, 128]), ("attn_w_o", [D, D]), ("sgu_norm", [1, D]), ("sgu_w_uv", [D, 2048]),
    ("sgu_ln_g", [1, D]), ("sgu_ln_b", [1, D]), ("sgu_w_s", [8, 128, 128]), ("sgu_b_s", [8, 128]),
    ("sgu_w_out", [D, D]), ("ffn_norm", [2, D]), ("ffn_w_up", [2, D, 2 * DFF]), ("ffn_conv_w", [2, 3, 2 * DFF]),
    ("ffn_conv_b", [2, 2 * DFF]), ("ffn_w_down", [2, DFF, D]), ("final_norm", [1, D]),
]
CONST_SPECS = [
    ("identc", [128, 128]), ("tril", [128, 128]), ("triu", [128, 128]), ("gU", [128, 128]), ("gM1", [128, 128]),
    ("gLs", [128, 128]), ("cbias", [128, 128]), ("kbias", [128, NB]), ("pos", [128, NB]), ("invf", [1, 8]),
    ("pw", [1, NBIS + 1]), ("halo", [128, 1]), ("rowmask", [128, 4]),
]


def host_consts(p):
    i = np.arange(128)
    c = {}
    c["identc"] = np.eye(128, dtype=np.float32)
    c["tril"] = (i[None, :] <= i[:, None]).astype(np.float32)
    c["triu"] = (i[:, None] <= i[None, :]).astype(np.float32)
    sc = np.float32(-1.0 / 16.0)
    U = (i[:, None] <= i[None, :]).astype(np.float32)
    c["gU"] = U * sc
    c["gM1"] = (U - U[:, 64:65]) * sc
    c["gLs"] = (i[:, None] > i[None, :]).astype(np.float32) * sc
    c["cbias"] = np.where(i[None, :] <= i[:, None], 0.0, NEG).astype(np.float32)
    kb = np.zeros((128, NB), np.float32)
    lpos = np.arange(NB * 128)
    if p == 0:
        kb[:, :16] = NEG
        gpos = np.maximum(lpos - 16 * 128, 0)
    else:
        gpos = lpos
    c["kbias"] = kb
    c["pos"] = gpos.reshape(NB, 128).T.astype(np.float32).copy()
    c["invf"] = np.power(np.float32(500000.0), -(np.arange(8, dtype=np.float32) * np.float32(2.0) / np.float32(16.0))).astype(np.float32)[None, :]
    pw = np.array([2.0 ** -(k + 1) for k in range(NBIS)] + [2.0 ** -NBIS], np.float32)
    c["pw"] = pw[None, :]
    c["halo"] = np.full((128, 1), float(p), np.float32)
    rm = np.zeros((128, 4), np.float32)
    rm[:64, 0] = 1.0
    rm[64:, 1] = 1.0
    rm[:64, 2] = 0.125
    rm[64:, 3] = 0.125
    c["rowmask"] = rm
    return c


def build(stages=("l0", "ffn0", "sgu", "ffn1"), l0_first_own=OWN0, l0_nblocks=NB):
    nc = bass.Bass("TRN2", target_bir_lowering=False)
    C = {}
    for name, shape in WEIGHT_SPECS + CONST_SPECS:
        C[name] = nc.dram_tensor(name, shape, F32, kind="ExternalInput").ap()
    for name in ("ffn_w_up", "ffn_conv_w", "ffn_conv_b", "ffn_w_down"):
        t = C[name]
        C[name] = [t[0], t[1]]
    t = C["ffn_norm"]
    C["ffn_norm"] = [t[0:1, :], t[1:2, :]]
    b = B(nc)
    full = tuple(stages) == ("l0", "ffn0", "sgu", "ffn1")
    if stages[0] == "l0":
        xin = nc.dram_tensor("xloc", [NB * 128, D], F32, kind="ExternalInput").ap()
    else:
        xin = nc.dram_tensor("xs_in", [NOWN * 128, D], F32, kind="ExternalInput").ap()
    if stages[-1] == "ffn1":
        out = nc.dram_tensor("out", [16 * 128, D], F32, kind="ExternalOutput").ap()
    else:
        out = nc.dram_tensor("xs_out", [NOWN * 128, D], F32, kind="ExternalOutput").ap()
    for nm in ("PA", "PB", "PC", "PD"):
        C[nm] = b.ps(nm, [128, 1024], F32)
    C["identf"] = b.sb("identf", [128, 128], F32)
    b.dma("sp", C["identf"], C["identc"], [], ["identf"])
    C["ident"] = b.sb("ident", [128, 128], BF16)
    b.cpv(C["ident"], C["identf"], ["identf"], ["ident"])
    trl = b.sb("tril", [128, 128], F32)
    b.dma("sp", trl, C["tril"], [], ["tril"])
    C["tril"] = trl
    hl = b.sb("halo", [128, 1], F32)
    b.dma("sp", hl, C["halo"], [], ["halo"])
    C["halo"] = hl
    rmk = b.sb("rowmask", [128, 4], F32)
    b.dma("sp", rmk, C["rowmask"], [], ["rowmask"])
    C["rowmask"] = rmk
    b.persist()
    cur, curk = xin, "d_in"
    for si, sname in enumerate(stages):
        lastst = si == len(stages) - 1
        dstt = out if lastst else nc.dram_tensor("xs_%s" % sname, [NOWN * 128, D], F32).ap()
        dk = "d_%s" % sname
        b.new_stage()
        if sname == "l0":
            stage_l0(b, C, cur, dstt, dk, first_own=l0_first_own, nblocks=l0_nblocks)
        elif sname == "ffn0":
            stage_ffn(b, C, 0, cur, dstt, 1, 17, False, "f0", curk, dk)
        elif sname == "sgu":
            stage_sgu(b, C, cur, dstt, curk, dk)
        elif sname == "ffn1":
            stage_ffn(b, C, 1, cur, dstt, 2, 16, True, "f1", curk, dk)
        cur, curk = dstt, dk
    counts = b.S.emit()
    global LAST_B
    LAST_B = b
    return nc, counts


def core_inputs(inputs, c):
    bb, p = c // 2, c % 2
    m = {}
    for name, shape in WEIGHT_SPECS:
        m[name] = np.ascontiguousarray(np.asarray(inputs[name], dtype=np.float32).reshape(shape))
    m.update(host_consts(p))
    return m, bb, p


_NC_CACHE = {}


def kernel(**inputs):
    x = np.asarray(inputs["x"], dtype=np.float32)
    if "full" not in _NC_CACHE:
        _NC_CACHE["full"] = build()[0]
    nc = _NC_CACHE["full"]
    in_maps = []
    for c in range(8):
        m, bb, p = core_inputs(inputs, c)
        xl = np.zeros((NB * 128, D), np.float32)
        if p == 1:
            xl[:] = x[bb]
        else:
            xl[16 * 128:] = x[bb, :16 * 128]
        m["xloc"] = xl
        in_maps.append(m)
    res = run_bass_kernel_spmd(nc, in_maps, core_ids=list(range(8)))
    out = np.zeros((4, 4096, D), np.float32)
    for c in range(8):
        bb, p = c // 2, c % 2
        out[bb, p * 2048:(p + 1) * 2048] = res.results[c]["out"]
    return out
```

```python
import contextlib
import os
import numpy as np
import concourse.bass as bass
import concourse.mybir as mybir
from concourse.bass_utils import run_bass_kernel_spmd

F32 = mybir.dt.float32
BF16 = mybir.dt.bfloat16
I32 = mybir.dt.int32
AF = mybir.ActivationFunctionType
ALU = mybir.AluOpType
AX = mybir.AxisListType

ENGS = ("pe", "act", "dve", "pool", "sp")
NEG = -30000.0
NB = 32
OWN0 = 14
NOWN = NB - OWN0
D = 1024
DFF = 2816
NBIS = 16


class _Op:
    __slots__ = ("eng", "fn", "reads", "writes", "idx", "deps", "need_inc",
                 "milestone", "is_dma", "dsem", "dval", "dprev")


class Sched:
    NDMA = 8

    def __init__(self, nc):
        self.nc = nc
        self.ops = []
        self.last_w = {}
        self.readers = {}
        self.bar = set()
        self.mute = False

    def barrier(self):
        last = {}
        dl = {}
        for op in self.ops:
            if op.is_dma:
                dl.setdefault(op.eng, []).append(op.idx)
            else:
                last[op.eng] = op.idx
        bar = set(last.values())
        for e, l in dl.items():
            bar.update(l[-self.NDMA:])
        self.bar = bar

    def _add(self, eng, fn, reads, writes, is_dma):
        if self.mute:
            return None
        op = _Op()
        op.eng, op.fn, op.reads, op.writes, op.is_dma = eng, fn, tuple(reads), tuple(writes), is_dma
        op.need_inc = False
        op.milestone = 0
        op.dsem = None
        op.dval = 0
        op.dprev = 0
        op.idx = len(self.ops)
        deps = set()
        for k in op.reads:
            w = self.last_w.get(k)
            if w is not None:
                deps.add(w)
        for k in op.writes:
            w = self.last_w.get(k)
            if w is not None:
                deps.add(w)
            for r in self.readers.get(k, ()):
                deps.add(r)
        deps.update(self.bar)
        deps.discard(op.idx)
        op.deps = deps
        for k in op.reads:
            self.readers.setdefault(k, []).append(op.idx)
        for k in op.writes:
            self.last_w[k] = op.idx
            self.readers[k] = []
        self.ops.append(op)
        return op

    def pe(self, fn, reads=(), writes=()):
        return self._add("pe", fn, reads, writes, False)

    def act(self, fn, reads=(), writes=()):
        return self._add("act", fn, reads, writes, False)

    def dve(self, fn, reads=(), writes=()):
        return self._add("dve", fn, reads, writes, False)

    def dma(self, queue, fn, reads=(), writes=()):
        return self._add(queue, fn, reads, writes, True)

    @staticmethod
    def _hazard(p, op):
        return bool((set(p.writes) & (set(op.reads) | set(op.writes))) or (set(p.reads) & set(op.writes)))

    def emit(self):
        nc = self.nc
        ops = self.ops
        for op in ops:
            for d in op.deps:
                p = ops[d]
                if p.is_dma:
                    continue
                if p.eng == op.eng and not op.is_dma:
                    if p.eng == "pe" or not self._hazard(p, op):
                        continue
                p.need_inc = True
        per_eng = {e: [] for e in ENGS}
        for op in ops:
            per_eng[op.eng].append(op)
        dcnt = {}
        for e in ENGS:
            n = 0
            for op in per_eng[e]:
                if op.is_dma:
                    k = dcnt.get(e, 0)
                    dcnt[e] = k + 1
                    op.dsem = (e, k % self.NDMA)
                    op.dprev = 16 * (k // self.NDMA)
                    op.dval = op.dprev + 16
                elif op.need_inc:
                    n += 1
                    op.milestone = n
        with contextlib.ExitStack() as es:
            esem = {e: es.enter_context(nc.semaphore("ms_" + e)) for e in ENGS}
            dsem = {}
            for e in dcnt:
                for k in range(min(self.NDMA, dcnt[e])):
                    dsem[(e, k)] = es.enter_context(nc.semaphore("dq_%s_%d" % (e, k)))
            block = es.enter_context(nc.Block())

            def run(e, eng):
                seen = {}
                for op in per_eng[e]:
                    need = {}
                    for d in op.deps:
                        p = ops[d]
                        if p.is_dma:
                            need[p.dsem] = max(need.get(p.dsem, 0), p.dval)
                        else:
                            if not p.need_inc:
                                continue
                            if p.eng == e and not op.is_dma:
                                if e == "pe" or not self._hazard(p, op):
                                    continue
                            need[p.eng] = max(need.get(p.eng, 0), p.milestone)
                    if op.is_dma and op.dprev > 0:
                        need[op.dsem] = max(need.get(op.dsem, 0), op.dprev)
                    for key, val in need.items():
                        if seen.get(key, 0) >= val:
                            continue
                        seen[key] = val
                        eng.wait_ge(dsem[key] if isinstance(key, tuple) else esem[key], val)
                    ins = op.fn(eng)
                    if op.is_dma:
                        ins.then_inc(dsem[op.dsem], 16)
                    elif op.need_inc:
                        ins.then_inc(esem[e], 1)
                if e in dcnt:
                    last = {}
                    for op in per_eng[e]:
                        if op.is_dma:
                            last[op.dsem] = op.dval
                    for key, val in last.items():
                        if seen.get(key, 0) < val:
                            eng.wait_ge(dsem[key], val)

            if per_eng["pe"]:
                block.tensor(lambda eng: run("pe", eng))
            if per_eng["act"]:
                block.scalar(lambda eng: run("act", eng))
            if per_eng["dve"]:
                block.vector(lambda eng: run("dve", eng))
            if per_eng["pool"]:
                block.gpsimd(lambda eng: run("pool", eng))
            if per_eng["sp"]:
                block.sync(lambda eng: run("sp", eng))
        return {e: len(per_eng[e]) for e in ENGS}


class B:
    ARENA_WORDS = 53000

    def __init__(self, nc):
        self.nc = nc
        self.S = Sched(nc)
        self.n = 0
        self.arena = nc.alloc_sbuf_tensor("arena", [128, self.ARENA_WORDS], F32).ap()
        self.off = 0
        self.mark = 0

    def sb(self, name, shape, dt):
        esz = {F32: 4, BF16: 2, I32: 4}[dt]
        nel = int(np.prod(shape[1:]))
        words = (nel * esz + 31) // 32 * 8
        assert self.off + words <= self.ARENA_WORDS, "SBUF arena overflow at %s: %d" % (name, self.off + words)
        ap = self.arena[0:shape[0], self.off:self.off + words]
        if not hasattr(self, "names"):
            self.names = {}
        self.names[name] = (self.off, tuple(shape), dt)
        self.off += words
        if dt != F32:
            ap = ap.bitcast(dt)
        ap = ap[:, 0:nel]
        if len(shape) == 3:
            ap = ap.rearrange("p (a b) -> p a b", b=shape[2])
        elif len(shape) == 4:
            ap = ap.rearrange("p (a b c) -> p a b c", b=shape[2], c=shape[3])
        return ap

    def persist(self):
        self.mark = self.off

    def new_stage(self):
        self.off = self.mark
        self.S.barrier()

    def ps(self, name, shape, dt):
        self.n += 1
        return self.nc.alloc_psum_tensor("%s_%d" % (name, self.n), list(shape), dt).ap()

    def mm(self, out, lhsT, rhs, start, stop, r, w):
        self.S.pe(lambda e: e.matmul(out, lhsT=lhsT, rhs=rhs, start=start, stop=stop, skip_group_check=True), r, w)

    def tr(self, out, in_, ident, r, w):
        self.S.pe(lambda e: e.transpose(out=out, in_=in_, identity=ident), list(r) + ["ident", "identf"], w)

    def act(self, out, in_, func, r, w, bias=None, scale=None, accum_out=None):
        kw = {}
        if bias is not None:
            kw["bias"] = bias
        if scale is not None:
            kw["scale"] = scale
        if accum_out is not None:
            kw["accum_out"] = accum_out
        self.S.act(lambda e: e.activation(out=out, in_=in_, func=func, **kw), r, w)

    def ts(self, out, in0, s1, s2, op0, op1, r, w, accum_out=None):
        kw = {}
        if accum_out is not None:
            kw["accum_out"] = accum_out
        if op1 is None:
            self.S.dve(lambda e: e.tensor_scalar(out=out, in0=in0, scalar1=s1, scalar2=None, op0=op0, **kw), r, w)
        else:
            self.S.dve(lambda e: e.tensor_scalar(out=out, in0=in0, scalar1=s1, scalar2=s2, op0=op0, op1=op1, **kw), r, w)

    def tt(self, out, in0, in1, op, r, w):
        self.S.dve(lambda e: e.tensor_tensor(out=out, in0=in0, in1=in1, op=op), r, w)

    def stt(self, out, in0, scalar, in1, op0, op1, r, w):
        self.S.dve(lambda e: e.scalar_tensor_tensor(out=out, in0=in0, scalar=scalar, in1=in1, op0=op0, op1=op1), r, w)

    def cpv(self, out, in_, r, w):
        self.S.dve(lambda e: e.tensor_copy(out=out, in_=in_), r, w)

    def cpa(self, out, in_, r, w):
        self.S.act(lambda e: e.copy(out=out, in_=in_), r, w)

    def memset(self, ap, val, w):
        self.S.dve(lambda e: e.memset(ap, val), (), w)

    def recip(self, out, in_, r, w):
        self.S.dve(lambda e: e.reciprocal(out=out, in_=in_), r, w)

    def dma(self, q, out, in_, r, w):
        self.S.dma(q, lambda e: e.dma_start(out=out, in_=in_), r, w)

    def rmsnorm(self, x, xk, gbc, gk, h, hk, st, stk, junk, junkk, n=D, eps=1e-6):
        self.act(junk, x, AF.Square, [xk], [junkk, stk], accum_out=st[:, 0:1])
        self.ts(st[:, 1:2], st[:, 0:1], 1.0 / n, eps, ALU.mult, ALU.add, [stk], [stk])
        self.act(st[:, 2:3], st[:, 1:2], AF.Sqrt, [stk], [stk])
        self.recip(st[:, 3:4], st[:, 2:3], [stk], [stk])
        self.stt(h, x, st[:, 3:4], gbc, ALU.mult, ALU.mult, [xk, stk, gk], [hk])


def load_w(b, q, dst, src2d, c0, c1, d0, wkey):
    v = src2d.rearrange("(k p) n -> p k n", p=128)
    b.dma(q, dst[:, :, d0:d0 + (c1 - c0)], v[:, :, c0:c1], [], [wkey])


def stage_ffn(b, C, li, src, dst, ob0, nblk, final, tag, sk, dk):
    nc = b.nc
    T = lambda s: "%s_%s" % (tag, s)
    w_up, conv_w, conv_b, w_dn, gnorm = C["ffn_w_up"][li], C["ffn_conv_w"][li], C["ffn_conv_b"][li], C["ffn_w_down"][li], C["ffn_norm"][li]
    ident, identf = C["ident"], C["identf"]
    PA, PB, PC, PD = C["PA"], C["PB"], C["PC"], C["PD"]
    wdn = b.sb("wdn", [128, 22, D], BF16)
    b.dma("pool", wdn, w_dn.rearrange("(k p) n -> p k n", p=128), [], [T("wdn")])
    gbc = b.sb("gbc", [128, D], F32)
    b.dma("sp", gbc, gnorm.partition_broadcast(128), [], [T("gbc")])
    raw = b.sb("cwraw", [44, 4, 128], F32)
    for j in range(3):
        b.dma("sp", raw[:, j, :], conv_w[j].rearrange("(c p) -> c p", p=128), [], [T("raw")])
    b.dma("sp", raw[:, 3, :], conv_b.rearrange("(c p) -> c p", p=128), [], [T("raw")])
    cwb = b.sb("cwb", [128, 4, 44], F32)
    pv = PB[:, 0:176].rearrange("p (a c) -> p a c", c=44)
    for j in range(4):
        b.tr(pv[:, j, :], raw[:, j, :], identf[0:44, 0:44], [T("raw")], ["P2"])
    b.cpv(cwb, pv, ["P2"], [T("cwb")])
    if final:
        fbc = b.sb("fbc", [128, D], F32)
        b.dma("sp", fbc, C["final_norm"].partition_broadcast(128), [], [T("fbc")])

    blocks = [ob0 - 1] + list(range(ob0, ob0 + nblk))
    sts = []
    i = 0
    while i < len(blocks):
        sts.append(blocks[i:i + 9])
        i += 9
    NTMAX = 9 * 128
    hT2 = [b.sb("hT", [128, 8, NTMAX], BF16) for _ in range(2)]
    g = b.sb("g", [128, 22, NTMAX], BF16)
    ahalo = b.sb("ahalo", [128, 44, 2], BF16)
    b.memset(ahalo, 0.0, [T("ahalo")])
    xt = [b.sb("xt", [128, D], F32) for _ in range(2)]
    hb = [b.sb("hb", [128, D], BF16) for _ in range(2)]
    st = [b.sb("st", [128, 4], F32) for _ in range(2)]
    junk = b.sb("junk", [128, D], F32)
    wbuf = [b.sb("wbuf", [128, 8, 512], BF16) for _ in range(3)]
    a_sb = [b.sb("a_sb", [128, 2 + 512], BF16) for _ in range(2)]
    sg = b.sb("sg", [128, 9 * 128], F32)
    dg = [[b.sb("dg", [128, 3, 128], BF16) for _ in range(2)] for _ in range(2)]
    xo = [b.sb("xo", [128, D], F32) for _ in range(2)]
    ho = b.sb("ho", [128, D], F32)
    halo_flag = C["halo"]
    nw = 0
    def norm_block(sti, bi, ob):
        hT = hT2[sti % 2]
        s = bi % 2
        b.dma("sp", xt[s], src[ob * 128:(ob + 1) * 128, :], ["%s_%d" % (sk, ob)], [T("xt%d" % s)])
        b.rmsnorm(xt[s], T("xt%d" % s), gbc, T("gbc"), hb[s], T("hb%d" % s), st[s], T("st%d" % s), junk, T("junk"))
        pt = PB[:, 0:512].bitcast(BF16).rearrange("p (k t) -> p k t", t=128)
        for k in range(8):
            b.tr(pt[:, k, :], hb[s][:, k * 128:(k + 1) * 128], ident, [T("hb%d" % s)], ["P2"])
        if sti == 0 and bi == 0:
            b.ts(hT[:, :, bi * 128:(bi + 1) * 128], pt, halo_flag[:, 0:1], None, ALU.mult, None, ["P2", "halo"], [T("hT%d" % (sti % 2))])
        else:
            b.cpa(hT[:, :, bi * 128:(bi + 1) * 128], pt, ["P2"], [T("hT%d" % (sti % 2))])

    for bi, ob in enumerate(sts[0]):
        norm_block(0, bi, ob)
    for sti, blks in enumerate(sts):
        NT = 128 * len(blks)
        hT = hT2[sti % 2]
        hTk = T("hT%d" % (sti % 2))
        nxt = list(enumerate(sts[sti + 1])) if sti + 1 < len(sts) else []
        pieces = []
        c = 0
        while c < NT:
            pieces.append((c, min(512, NT - c)))
            c += 512
        pend = [None]
        uc = [0]

        def unit_U(ws, cc, half, ch, c0, n, u):
            pa = (PA[:, 0:512], "P0") if u % 2 == 0 else (PA[:, 512:1024], "P1")
            for k in range(8):
                b.mm(pa[0][:, 0:n], wbuf[ws][:, k, half * 256 + cc * 128: half * 256 + (cc + 1) * 128],
                     hT[:, k, c0:c0 + n], k == 0, k == 7, [T("wbuf%d" % ws), hTk], [pa[1]])
            asb = a_sb[u % 2]
            ak = T("asb%d" % (u % 2))
            b.cpv(asb[:, 0:2], ahalo[:, ch, :], [T("ahalo")], [ak])
            b.cpa(asb[:, 2:2 + n], pa[0][:, 0:n], [pa[1]], [ak])
            b.cpv(ahalo[:, ch, :], asb[:, n:n + 2], [ak], [T("ahalo")])

        def unit_V(fch, half, ch, c0, n, u, dgt, dgk):
            pc = (PC[:, 0:512], "P4") if u % 2 == 0 else (PC[:, 512:1024], "P5")
            asb = a_sb[u % 2]
            ak = T("asb%d" % (u % 2))
            for tap in range(3):
                b.mm(pc[0][:, 0:n], dgt[:, tap, :], asb[:, tap:tap + n], tap == 0, tap == 2, [dgk, ak], [pc[1]])
            if half == 0:
                b.act(sg[:, c0:c0 + n], pc[0][:, 0:n], AF.Silu, [pc[1], T("cwb")], [T("sg")], bias=cwb[:, 3, ch:ch + 1])
            else:
                b.stt(g[:, fch, c0:c0 + n], pc[0][:, 0:n], cwb[:, 3, ch:ch + 1], sg[:, c0:c0 + n], ALU.add, ALU.mult,
                      [pc[1], T("cwb"), T("sg")], [T("g")])

        for jj in range(11):
            if jj < len(nxt):
                norm_block(sti + 1, nxt[jj][0], nxt[jj][1])
            ws = nw % 3
            nw += 1
            wv = w_up.rearrange("(k p) n -> p k n", p=128)
            b.dma("pool", wbuf[ws][:, :, 0:256], wv[:, :, jj * 256:(jj + 1) * 256], [], [T("wbuf%d" % ws)])
            b.dma("pool", wbuf[ws][:, :, 256:512], wv[:, :, DFF + jj * 256:DFF + (jj + 1) * 256], [], [T("wbuf%d" % ws)])
            for cc in range(2):
                fch = jj * 2 + cc
                par = fch % 2
                for half in range(2):
                    ch = fch + 22 * half
                    for tap in range(3):
                        b.ts(dg[par][half][:, tap, :], ident, cwb[:, tap, ch:ch + 1], None, ALU.mult, None,
                             [T("cwb"), "ident"], [T("dg%d_%d" % (par, half))])
                for (c0, n) in pieces:
                    for half in range(2):
                        ch = fch + 22 * half
                        u = uc[0]
                        uc[0] += 1
                        unit_U(ws, cc, half, ch, c0, n, u)
                        if pend[0] is not None:
                            unit_V(*pend[0])
                        pend[0] = (fch, half, ch, c0, n, u, dg[par][half], T("dg%d_%d" % (par, half)))
        if pend[0] is not None:
            unit_V(*pend[0])
            pend[0] = None
        for bi, ob in enumerate(blks):
            if sti == 0 and bi == 0:
                continue
            s = bi % 2
            b.dma("sp", xt[s], src[ob * 128:(ob + 1) * 128, :], ["%s_%d" % (sk, ob)], [T("xt%d" % s)])
            PDx, pk0, pk1 = (PD, "P6", "P7") if bi % 2 == 0 else (PB, "P2", "P3")
            for half in range(2):
                for j in range(22):
                    b.mm(PDx[:, half * 512:(half + 1) * 512], g[:, j, bi * 128:(bi + 1) * 128], wdn[:, j, half * 512:(half + 1) * 512],
                         j == 0, j == 21, [T("g"), T("wdn")], [pk0 if half == 0 else pk1])
            b.tt(xo[s], PDx, xt[s], ALU.add, [pk0, pk1, T("xt%d" % s)], [T("xo%d" % s)])
            if not final:
                b.dma("sp", dst[ob * 128:(ob + 1) * 128, :], xo[s], [T("xo%d" % s)], ["%s_%d" % (dk, ob)])
            else:
                b.act(junk, xo[s], AF.Square, [T("xo%d" % s)], [T("junk"), T("st%d" % s)], accum_out=st[s][:, 0:1])
                b.ts(st[s][:, 1:2], st[s][:, 0:1], 1.0 / D, 1e-6, ALU.mult, ALU.add, [T("st%d" % s)], [T("st%d" % s)])
                b.act(st[s][:, 2:3], st[s][:, 1:2], AF.Sqrt, [T("st%d" % s)], [T("st%d" % s)])
                b.recip(st[s][:, 3:4], st[s][:, 2:3], [T("st%d" % s)], [T("st%d" % s)])
                b.stt(ho, xo[s], st[s][:, 3:4], fbc, ALU.mult, ALU.mult, [T("xo%d" % s), T("st%d" % s), T("fbc")], [T("ho")])
                b.dma("sp", dst[(ob - 2) * 128:(ob - 1) * 128, :], ho, [T("ho")], ["%s_%d" % (dk, ob)])


def stage_sgu(b, C, src, dst, sk, dk, tag="sgu"):
    T = lambda s: "%s_%s" % (tag, s)
    ident, identf = C["ident"], C["identf"]
    PA, PB, PC, PD = C["PA"], C["PB"], C["PC"], C["PD"]
    wuv = b.sb("wuv", [128, 8, 2048], BF16)
    load_w(b, "pool", wuv, C["sgu_w_uv"], 0, 2048, 0, T("wuv"))
    wout = b.sb("wout", [128, 8, D], BF16)
    load_w(b, "pool", wout, C["sgu_w_out"], 0, D, 0, T("wout"))
    gbc = b.sb("gbc", [128, D], F32)
    b.dma("sp", gbc, C["sgu_norm"].partition_broadcast(128), [], [T("gbc")])
    lng = b.sb("lng", [128, D], F32)
    b.dma("sp", lng, C["sgu_ln_g"].partition_broadcast(128), [], [T("lng")])
    lnb = b.sb("lnb", [128, D], F32)
    b.dma("sp", lnb, C["sgu_ln_b"].partition_broadcast(128), [], [T("lnb")])
    wsr = b.sb("wsr", [128, 8, 128], F32)
    b.dma("sp", wsr, C["sgu_w_s"].rearrange("g t s -> t g s"), [], [T("wsr")])
    wsm = b.sb("wsm", [128, 8, 128], BF16)
    b.tt(wsm, wsr, C["tril"].unsqueeze(1).to_broadcast([128, 8, 128]), ALU.mult, [T("wsr"), "tril"], [T("wsm")])
    wsT = b.sb("wsT", [128, 8, 128], BF16)
    pt = PB[:, 0:512].bitcast(BF16).rearrange("p (k t) -> p k t", t=128)
    for gi in range(8):
        b.tr(pt[:, gi, :], wsm[:, gi, :], ident, [T("wsm")], ["P2"])
    b.cpv(wsT, pt, ["P2"], [T("wsT")])
    bsr = b.sb("bsr", [8, 128], F32)
    b.dma("sp", bsr, C["sgu_b_s"], [], [T("bsr")])
    bsT = b.sb("bsT", [128, 8], F32)
    b.tr(PB[:, 512:520], bsr, identf[0:8, 0:8], [T("bsr")], ["P3"])
    b.cpv(bsT, PB[:, 512:520], ["P3"], [T("bsT")])

    xt = [b.sb("xt", [128, D], F32) for _ in range(3)]
    hb = b.sb("hb", [128, D], BF16)
    st = b.sb("st", [128, 8], F32)
    junk = b.sb("junk", [128, D], F32)
    hT = b.sb("hT", [128, 8, 128], BF16)
    u2 = [b.sb("u", [128, D], F32) for _ in range(2)]
    v = b.sb("v", [128, D], F32)
    v1 = b.sb("v1", [128, D], F32)
    vn2 = [b.sb("vn", [128, D], BF16) for _ in range(2)]
    gt = b.sb("gt", [128, D], BF16)
    gtT = b.sb("gtT", [128, 8, 128], BF16)
    xo = [b.sb("xo", [128, D], F32) for _ in range(2)]

    def p1(ob):
        s = ob % 2
        sx = ob % 3
        u, vn = u2[s], vn2[s]
        uk, vnk = T("u%d" % s), T("vn%d" % s)
        b.dma("sp", xt[sx], src[ob * 128:(ob + 1) * 128, :], ["%s_%d" % (sk, ob)], [T("xt%d" % sx)])
        b.rmsnorm(xt[sx], T("xt%d" % sx), gbc, T("gbc"), hb, T("hb"), st, T("st"), junk, T("junk"))
        for k in range(8):
            b.tr(pt[:, k, :], hb[:, k * 128:(k + 1) * 128], ident, [T("hb")], ["P2"])
        b.cpa(hT, pt, ["P2"], [T("hT")])
        zb = [(PA[:, 0:512], "P0"), (PA[:, 512:1024], "P1"), (PC[:, 0:512], "P4"), (PC[:, 512:1024], "P5")]
        for gi in range(4):
            for k in range(8):
                b.mm(zb[gi][0], hT[:, k, :], wuv[:, k, gi * 512:(gi + 1) * 512], k == 0, k == 7, [T("hT"), T("wuv")], [zb[gi][1]])
        b.act(u[:, 0:512], zb[0][0], AF.Gelu_apprx_tanh, ["P0"], [uk])
        b.act(u[:, 512:1024], zb[1][0], AF.Gelu_apprx_tanh, ["P1"], [uk])
        b.act(v[:, 0:512], zb[2][0], AF.Gelu_apprx_tanh, ["P4"], [T("v"), T("st")], accum_out=st[:, 4:5])
        b.act(v[:, 512:1024], zb[3][0], AF.Gelu_apprx_tanh, ["P5"], [T("v"), T("st")], accum_out=st[:, 5:6])
        b.act(junk, v, AF.Square, [T("v")], [T("junk"), T("st")], accum_out=st[:, 6:7])
        b.tt(st[:, 4:5], st[:, 4:5], st[:, 5:6], ALU.add, [T("st")], [T("st")])
        b.ts(st[:, 4:5], st[:, 4:5], 1.0 / D, None, ALU.mult, None, [T("st")], [T("st")])
        b.tt(st[:, 5:6], st[:, 4:5], st[:, 4:5], ALU.mult, [T("st")], [T("st")])
        b.stt(st[:, 6:7], st[:, 6:7], 1.0 / D, st[:, 5:6], ALU.mult, ALU.subtract, [T("st")], [T("st")])
        b.ts(st[:, 6:7], st[:, 6:7], 1e-5, None, ALU.add, None, [T("st")], [T("st")])
        b.act(st[:, 7:8], st[:, 6:7], AF.Sqrt, [T("st")], [T("st")])
        b.recip(st[:, 7:8], st[:, 7:8], [T("st")], [T("st")])
        b.stt(v1, v, st[:, 4:5], lng, ALU.subtract, ALU.mult, [T("v"), T("st"), T("lng")], [T("v1")])
        b.stt(vn, v1, st[:, 7:8], lnb, ALU.mult, ALU.add, [T("v1"), T("st"), T("lnb")], [vnk])

    def p2(ob):
        s = ob % 2
        sx = ob % 3
        u, vn = u2[s], vn2[s]
        uk, vnk = T("u%d" % s), T("vn%d" % s)
        for gi in range(8):
            b.mm(PD[:, gi * 128:(gi + 1) * 128], wsT[:, gi, :], vn[:, gi * 128:(gi + 1) * 128], True, True,
                 [T("wsT"), vnk], ["P6" if gi < 4 else "P7"])
        for gi in range(8):
            b.stt(gt[:, gi * 128:(gi + 1) * 128], PD[:, gi * 128:(gi + 1) * 128], bsT[:, gi:gi + 1], u[:, gi * 128:(gi + 1) * 128],
                  ALU.add, ALU.mult, ["P6" if gi < 4 else "P7", T("bsT"), uk], [T("gt")])
        for k in range(8):
            b.tr(pt[:, k, :], gt[:, k * 128:(k + 1) * 128], ident, [T("gt")], ["P2"])
        b.cpa(gtT, pt, ["P2"], [T("gtT")])
        for half in range(2):
            for k in range(8):
                b.mm(PA[:, half * 512:(half + 1) * 512], gtT[:, k, :], wout[:, k, half * 512:(half + 1) * 512], k == 0, k == 7,
                     [T("gtT"), T("wout")], ["P0" if half == 0 else "P1"])
        b.tt(xo[s], PA, xt[sx], ALU.add, ["P0", "P1", T("xt%d" % sx)], [T("xo%d" % s)])
        b.dma("sp", dst[ob * 128:(ob + 1) * 128, :], xo[s], [T("xo%d" % s)], ["%s_%d" % (dk, ob)])

    p1(1)
    for ob in range(1, NOWN):
        if ob + 1 < NOWN:
            p1(ob + 1)
        p2(ob)


def stage_l0(b, C, xloc, dst, dk, tag="l0", first_own=OWN0, nblocks=NB):
    T = lambda s: "%s_%s" % (tag, s)
    ident, identf = C["ident"], C["identf"]
    PA, PB, PC, PD = C["PA"], C["PB"], C["PC"], C["PD"]
    w_in = C["attn_w_in"]
    wk = b.sb("wk", [128, 8, 468], BF16)
    for (c0, c1, d0) in ((256, 512, 0), (1536, 1552, 256), (2064, 2128, 272), (2448, 2512, 336), (2128, 2192, 400), (2512, 2516, 464)):
        load_w(b, "pool", wk, w_in, c0, c1, d0, T("wk"))
    wv = b.sb("wv", [128, 8, 512], BF16)
    load_w(b, "pool", wv, w_in, 512, 1024, 0, T("wv"))
    wq = b.sb("wq", [128, 8, 1536], BF16)
    for (c0, c1, d0) in ((0, 256, 0), (2192, 2448, 256), (1552, 2064, 512), (1024, 1536, 1024)):
        load_w(b, "pool", wq, w_in, c0, c1, d0, T("wq"))
    wo = b.sb("wo", [128, 8, D], BF16)
    load_w(b, "pool", wo, C["attn_w_o"], 0, D, 0, T("wo"))
    PCUT = int(os.environ.get("L0_PCUT", "99"))
    if PCUT <= 0:
        return
    wa2 = b.sb("wa2", [16, 256], BF16)
    b.dma("pool", wa2, C["gla_w_a2"], [], [T("wa2")])
    ba = b.sb("ba", [1, 256], BF16)
    b.dma("pool", ba, C["gla_b_a"], [], [T("ba")])
    hg = b.sb("hg", [128, 128], F32)
    b.dma("sp", hg, C["gla_head_g"].partition_broadcast(128), [], [T("hg")])
    gbc = b.sb("gbc", [128, D], F32)
    b.dma("sp", gbc, C["attn_norm"].partition_broadcast(128), [], [T("gbc")])
    ones1 = b.sb("ones1", [1, 128], BF16)
    b.memset(ones1, 1.0, [T("ones1")])
    kbias = b.sb("kbias", [128, NB], F32)
    b.dma("sp", kbias, C["kbias"], [], [T("kbias")])
    cb = b.sb("cb", [128, 128], BF16)
    b.dma("pool", cb, C["cbias"], [], [T("cb")])
    i4 = b.sb("i4", [128, 4, 128], BF16)
    for r4 in range(4):
        b.cpv(i4[:, r4, :], ident, ["ident"], [T("i4")])
    mUT = b.sb("mUT", [128, 128], F32)
    b.dma("sp", mUT, C["triu"], [], [T("mUT")])
    gU = b.sb("gU", [128, 128], BF16)
    b.dma("pool", gU, C["gU"], [], [T("gU")])
    gM1 = b.sb("gM1", [128, 128], BF16)
    b.dma("pool", gM1, C["gM1"], [], [T("gM1")])
    gLs = b.sb("gLs", [128, 128], BF16)
    b.dma("pool", gLs, C["gLs"], [], [T("gLs")])
    pw = b.sb("pw", [128, NBIS + 1], F32)
    b.dma("sp", pw, C["pw"].partition_broadcast(128), [], [T("pw")])
    if PCUT <= 1:
        return
    pos = b.sb("pos", [128, NB], F32)
    b.dma("sp", pos, C["pos"], [], [T("pos")])
    invf = b.sb("invf", [128, 8], F32)
    b.dma("sp", invf, C["invf"].partition_broadcast(128), [], [T("invf")])
    ang = b.sb("ang", [128, NB, 8], F32)
    rr = b.sb("rr", [128, NB, 8], F32)
    ki = b.sb("ki", [128, NB, 8], I32)
    kf = b.sb("kf", [128, NB, 8], F32)
    mfix = b.sb("mfix", [128, NB, 8], F32)
    cosT = b.sb("cosT", [128, NB, 8], F32)
    sinT = b.sb("sinT", [128, NB, 8], F32)
    TWO_PI = float(2 * np.pi)
    b.tt(ang, pos.unsqueeze(2).to_broadcast([128, NB, 8]), invf.unsqueeze(1).to_broadcast([128, NB, 8]), ALU.mult,
         [T("pos"), T("invf")], [T("ang")])
    b.ts(rr, ang, 1.0 / TWO_PI, None, ALU.mult, None, [T("ang")], [T("rr")])
    b.cpv(ki, rr, [T("rr")], [T("ki")])
    b.cpv(kf, ki, [T("ki")], [T("kf")])
    b.stt(rr, kf, -TWO_PI, ang, ALU.mult, ALU.add, [T("kf"), T("ang")], [T("rr")])

    def wrap(t, key):
        b.ts(mfix, t, float(np.pi), -TWO_PI, ALU.is_gt, ALU.mult, [key], [T("mfix")])
        b.tt(t, t, mfix, ALU.add, [key, T("mfix")], [key])
        b.ts(mfix, t, float(-np.pi), TWO_PI, ALU.is_lt, ALU.mult, [key], [T("mfix")])
        b.tt(t, t, mfix, ALU.add, [key, T("mfix")], [key])

    wrap(rr, T("rr"))
    b.act(sinT, rr, AF.Sin, [T("rr")], [T("sinT")])
    b.ts(rr, rr, float(np.pi / 2), None, ALU.add, None, [T("rr")], [T("rr")])
    wrap(rr, T("rr"))
    b.act(cosT, rr, AF.Sin, [T("rr")], [T("cosT")])

    if PCUT <= 2:
        return
    kkT = b.sb("kkT", [128, 2, NB * 128], BF16)
    bva = b.sb("bva", [128, NB, 66], BF16)
    b.memset(bva, 1.0, [T("bva")])
    if PCUT <= 3:
        return
    Sf = b.sb("Sf", [128, 2, 128], F32)
    b.memset(Sf, 0.0, [T("Sf")])
    if PCUT <= 4:
        return
    Sb = b.sb("Sb", [128, 2, 128], BF16)
    b.memset(Sb, 0.0, [T("Sb")])

    xt = [b.sb("xt", [128, D], F32) for _ in range(3)]
    hb = b.sb("hb", [128, D], BF16)
    st = b.sb("st", [128, 4], F32)
    junk = b.sb("junk", [128, D], F32)
    hT = b.sb("hT", [128, 8, 128], BF16)
    k1s = b.sb("k1s", [128, 468], F32)
    vb = b.sb("vb", [128, 512], BF16)
    q1s = b.sb("q1s", [128, 512], F32)
    q2s = b.sb("q2s", [128, 512], F32)
    sar = b.sb("sar", [128, 512], F32)
    kk = b.sb("kk", [128, 256], BF16)
    ropA = b.sb("ropA", [128, 8, 2, 8], F32)
    ropB = b.sb("ropB", [128, 8, 2, 8], F32)
    alrb = b.sb("alrb", [128, 16], BF16)
    alrT = b.sb("alrT", [16, 128], BF16)
    e1 = b.sb("e1", [128, 256], F32)
    l1 = b.sb("l1", [128, 256], F32)
    l1h = b.sb("l1h", [128, 256], BF16)
    l1r = b.sb("l1r", [128, 256], F32)
    l1l = b.sb("l1l", [128, 256], BF16)
    E0 = b.sb("E0", [128, 2, 128], F32)
    E1 = b.sb("E1", [128, 2, 128], F32)
    E2 = b.sb("E2", [128, 2, 128], F32)
    E3 = b.sb("E3", [128, 256], F32)
    khat = b.sb("khat", [128, 256], BF16)
    qkb = b.sb("qkb", [128, 512], BF16)
    qtl = b.sb("qtl", [128, 2, 128], BF16)
    ktlz = [b.sb("ktlz", [128, 2, 128], BF16) for _ in range(2)]
    qhz = [b.sb("qhz", [128, 2, 128], BF16) for _ in range(2)]
    rmk = C["rowmask"]
    attm = b.sb("attm", [128, 4, 128], BF16)
    osb = b.sb("osb", [128, 4, 128], F32)
    osq = b.sb("osq", [128, 4, 128], F32)
    gst = b.sb("gst", [128, 12], F32)
    Gg = b.sb("Gg", [128, 4, 128], F32)
    cat2 = [b.sb("cat", [128, D], BF16) for _ in range(2)]
    catT = b.sb("catT", [128, 8, 128], BF16)
    iqs = b.sb("iqs", [128, 256], F32)
    iqb = b.sb("iqb", [128, 256], BF16)
    bqb = b.sb("bqb", [128, 512], BF16)
    iqz = b.sb("iqz", [128, 2, 2, 128], BF16)
    bqz2 = [[b.sb("bqz", [128, 4, 128], BF16) for _ in range(2)] for _ in range(2)]
    wab = b.sb("wab", [128, 8], F32)
    sgn = b.sb("sgn", [128, 4], F32)
    sd = b.sb("sd", [128, 4, 128], BF16)
    Rr = [b.sb("Rr", [128, 512], BF16) for _ in range(4)]
    scr = b.sb("scr", [128, NB * 128], F32)
    mb2 = [b.sb("mb", [128, NB * 128], BF16) for _ in range(2)]
    bis = b.sb("bis", [128, 8], F32)
    stp = b.sb("stp", [128, NBIS + 1], F32)
    stp2 = b.sb("stp2", [128, NBIS + 1], F32)
    mid = [b.sb("mid", [128, 1], F32) for _ in range(2)]
    cnt = b.sb("cnt", [128, 1], F32)
    dd = b.sb("dd", [128, 1], F32)
    PT = [b.sb("PT", [128, 1024], BF16) for _ in range(2)]
    den = b.sb("den", [128, 8], F32)

    ptb = PB[:, 0:512].bitcast(BF16).rearrange("p (k t) -> p k t", t=128)

    def rope(srcv, dstv, H, blk, rk, wk_):
        cb_ = cosT[:, blk, :].unsqueeze(1).to_broadcast([128, H, 8])
        sb_ = sinT[:, blk, :].unsqueeze(1).to_broadcast([128, H, 8])
        x1 = srcv[:, :, 0:8]
        x2 = srcv[:, :, 8:16]
        A = ropA[:, 0:H]
        Bm = ropB[:, 0:H]
        b.tt(A[:, :, 0, :], x1, cb_, ALU.mult, rk + [T("cosT")], [T("ropA")])
        b.tt(A[:, :, 1, :], x2, cb_, ALU.mult, rk + [T("cosT")], [T("ropA")])
        b.tt(Bm[:, :, 0, :], x1, sb_, ALU.mult, rk + [T("sinT")], [T("ropB")])
        b.tt(Bm[:, :, 1, :], x2, sb_, ALU.mult, rk + [T("sinT")], [T("ropB")])
        b.tt(dstv[:, :, 0:8], A[:, :, 0, :], Bm[:, :, 1, :], ALU.subtract, [T("ropA"), T("ropB")], wk_)
        b.tt(dstv[:, :, 8:16], A[:, :, 1, :], Bm[:, :, 0, :], ALU.add, [T("ropA"), T("ropB")], wk_)
        b.cpa(dstv[:, :, 16:64], srcv[:, :, 16:64], rk, wk_)

    def part_A(blk):
        own = blk >= first_own
        s = blk % 2
        sx = blk % 3
        xk = T("xt%d" % sx)
        cat = cat2[s]
        bqz = bqz2[s]
        b.dma("sp", xt[sx], xloc[blk * 128:(blk + 1) * 128, :], [], [xk])
        b.rmsnorm(xt[sx], xk, gbc, T("gbc"), hb, T("hb"), st, T("st"), junk, T("junk"))
        for k in range(8):
            b.tr(ptb[:, k, :], hb[:, k * 128:(k + 1) * 128], ident, [T("hb")], ["P2"])
        b.cpa(hT, ptb, ["P2"], [T("hT")])
        for k in range(8):
            b.mm(PA[:, 0:468], hT[:, k, :], wk[:, k, :], k == 0, k == 7, [T("hT"), T("wk")], ["P0"])
        b.cpv(k1s, PA[:, 0:468], ["P0"], [T("k1s")])
        for k in range(8):
            b.mm(PA[:, 512:1024], hT[:, k, :], wv[:, k, :], k == 0, k == 7, [T("hT"), T("wv")], ["P1"])
        b.cpa(vb, PA[:, 512:1024], ["P1"], [T("vb")])
        if own:
            for k in range(8):
                b.mm(PA[:, 0:512], hT[:, k, :], wq[:, k, 0:512], k == 0, k == 7, [T("hT"), T("wq")], ["P0"])
            b.cpv(q1s, PA[:, 0:512], ["P0"], [T("q1s")])
            for k in range(8):
                b.mm(PA[:, 512:1024], hT[:, k, :], wq[:, k, 512:1024], k == 0, k == 7, [T("hT"), T("wq")], ["P1"])
            b.cpa(q2s, PA[:, 512:1024], ["P1"], [T("q2s")])
            for k in range(8):
                b.mm(PA[:, 0:512], hT[:, k, :], wq[:, k, 1024:1536], k == 0, k == 7, [T("hT"), T("wq")], ["P0"])
            b.act(sar, PA[:, 0:512], AF.Silu, ["P0"], [T("sar")])
        kkv = kk[:, 0:128].rearrange("p (h e) -> p h e", e=64)
        rope(k1s[:, 272:400].rearrange("p (h e) -> p h e", e=64), kkv, 2, blk, [T("k1s")], [T("kk")])
        b.cpv(kk[:, 128:192], kk[:, 64:128], [T("kk")], [T("kk2")])
        b.cpv(kk[:, 192:256], kk[:, 0:64], [T("kk")], [T("kk2")])
        b.tr(ptb[:, 0, :], kk[:, 0:128], ident, [T("kk")], ["P2"])
        b.tr(ptb[:, 1, :], kk[:, 128:256], ident, [T("kk"), T("kk2")], ["P2"])
        cs = slice(blk * 128, (blk + 1) * 128)
        bkk, ikk = T("bkT_%d" % blk), T("ikT_%d" % blk)
        b.cpv(kkT[:, :, cs], ptb[:, 0:2, :], ["P2"], [bkk, ikk])
        b.cpv(bva[:, blk, 0:64], k1s[:, 400:464], [T("k1s")], [T("bva_%d" % blk)])
        b.cpv(alrb, k1s[:, 256:272], [T("k1s")], [T("alrb")])
        b.tr(ptb[0:16, 2, :], alrb, ident, [T("alrb")], ["P2"])
        b.cpv(alrT, ptb[0:16, 2, :], ["P2"], [T("alrT")])
        zP = PB[:, 512:768]
        b.mm(zP, alrT, wa2, True, False, [T("alrT"), T("wa2")], ["P3"])
        b.mm(zP, ones1, ba, False, True, [T("ones1"), T("ba")], ["P3"])
        b.act(e1, zP, AF.Exp, ["P3"], [T("e1")], scale=-1.0)
        b.act(l1, e1, AF.Ln, [T("e1")], [T("l1")], bias=1.0)
        b.cpv(l1h, l1, [T("l1")], [T("l1h")])
        b.tt(l1r, l1, l1h, ALU.subtract, [T("l1"), T("l1h")], [T("l1r")])
        b.cpv(l1l, l1r, [T("l1r")], [T("l1l")])
        X3 = PB[:, 768:1024]
        b.mm(X3, gLs, l1h, True, False, [T("gLs"), T("l1h")], ["P3"])
        b.mm(X3, gLs, l1l, False, True, [T("gLs"), T("l1l")], ["P3"])
        b.act(E3, X3, AF.Exp, ["P3"], [T("E3")])
        b.tt(khat, k1s[:, 0:256], E3, ALU.mult, [T("k1s"), T("E3")], [T("khat")])
        X0 = PC[:, 0:256].rearrange("p (a t) -> p a t", t=128)
        X1 = PC[:, 256:512].rearrange("p (a t) -> p a t", t=128)
        for hp in range(2):
            b.mm(X0[:, hp, :], l1h[:, hp * 128:(hp + 1) * 128], gU, True, False, [T("l1h"), T("gU")], ["P4"])
            b.mm(X0[:, hp, :], l1l[:, hp * 128:(hp + 1) * 128], gU, False, True, [T("l1l"), T("gU")], ["P4"])
        b.act(E0, X0, AF.Exp, ["P4"], [T("E0")])
        if own:
            for hp in range(2):
                b.mm(X1[:, hp, :], l1h[:, hp * 128:(hp + 1) * 128], gM1, True, False, [T("l1h"), T("gM1")], ["P4"])
                b.mm(X1[:, hp, :], l1l[:, hp * 128:(hp + 1) * 128], gM1, False, True, [T("l1l"), T("gM1")], ["P4"])
            b.act(E1, X1, AF.Exp, ["P4"], [T("E1")])
            b.act(E2, X1, AF.Exp, ["P4"], [T("E2")], scale=-1.0)
            b.cpv(qkb[:, 0:256], q1s[:, 0:256], [T("q1s")], [T("qkb")])
            b.cpa(qkb[:, 256:512], k1s[:, 0:256], [T("k1s")], [T("qkb")])
            for c4 in range(4):
                b.tr(ptb[:, 4 + c4, :], qkb[:, c4 * 128:(c4 + 1) * 128], ident, [T("qkb")], ["P2"])
            b.stt(qtl, ptb[:, 4:6, :], 0.125, E1, ALU.mult, ALU.mult, ["P2", T("E1")], [T("qtl")])
            for h2_ in range(2):
                b.stt(qhz[h2_], ptb[:, 4:6, :], rmk[:, 2 + h2_:3 + h2_], E0, ALU.mult, ALU.mult, ["P2", T("E0"), "rowmask"], [T("qhat")])
                b.stt(ktlz[h2_], ptb[:, 6:8, :], rmk[:, h2_:h2_ + 1], E2, ALU.mult, ALU.mult, ["P2", T("E2"), "rowmask"], [T("ktl")])
            aT = PC[:, 512:1024].rearrange("p (h t) -> p h t", t=128)
            for h in range(4):
                hp, h2 = h // 2, h % 2
                rs = slice(h2 * 64, (h2 + 1) * 64)
                b.mm(aT[:, h, :], ktlz[h2][:, hp, :], qtl[:, hp, :], True, True, [T("ktl"), T("qtl")], ["P5"])
            b.tt(attm, aT, mUT.unsqueeze(1).to_broadcast([128, 4, 128]), ALU.mult, ["P5", T("mUT")], [T("attm")])
            oP = PD[:, 0:512].rearrange("p (h t) -> p h t", t=128)
            for h in range(4):
                hp, h2 = h // 2, h % 2
                rs = slice(h2 * 64, (h2 + 1) * 64)
                b.mm(oP[:, h, :], attm[:, h, :], vb[:, h * 128:(h + 1) * 128], True, False, [T("attm"), T("vb")], ["P6"])
                b.mm(oP[:, h, :], qhz[h2][:, hp, :], Sb[:, hp, :], False, True, [T("qhat"), T("Sb")], ["P6"])
            b.cpa(osb, oP, ["P6"], [T("osb")])
            b.tt(osq, osb, osb, ALU.mult, [T("osb")], [T("osq")])
            b.S.dve(lambda e: e.tensor_reduce(out=gst[:, 0:4], in_=osq, axis=AX.X, op=ALU.add), [T("osq")], [T("gst")])
            b.ts(gst[:, 4:8], gst[:, 0:4], 1.0 / 128, 1e-6, ALU.mult, ALU.add, [T("gst")], [T("gst")])
            b.act(gst[:, 8:12], gst[:, 4:8], AF.Sqrt, [T("gst")], [T("gst2")])
            b.recip(gst[:, 4:8], gst[:, 8:12], [T("gst2")], [T("gst")])
            b.tt(Gg, sar.rearrange("p (h t) -> p h t", t=128), hg.unsqueeze(1).to_broadcast([128, 4, 128]), ALU.mult,
                 [T("sar"), T("hg")], [T("Gg")])
            b.tt(osq, osb, gst[:, 4:8].unsqueeze(2).to_broadcast([128, 4, 128]), ALU.mult, [T("osb"), T("gst")], [T("osq")])
            b.tt(cat[:, 0:512].rearrange("p (h t) -> p h t", t=128), osq, Gg, ALU.mult, [T("osq"), T("Gg")], [T("cat%d" % (blk % 2))])
        dS = PD[:, 512:1024].rearrange("p (a t) -> p a t", t=256)
        for hp in range(2):
            b.mm(dS[:, hp, :], khat[:, hp * 128:(hp + 1) * 128], vb[:, hp * 256:(hp + 1) * 256], True, True,
                 [T("khat"), T("vb")], ["P7"])
        dSs = osq.rearrange("p h t -> p (h t)").rearrange("p (a t) -> p a t", t=256)
        b.cpa(dSs, dS, ["P7"], [T("osq")])
        for hp in range(2):
            for h2 in range(2):
                rs = slice(h2 * 64, (h2 + 1) * 64)
                b.stt(Sf[rs, hp, :], Sf[rs, hp, :], E0[rs, hp, 127:128], dSs[rs, hp, h2 * 128:(h2 + 1) * 128], ALU.mult, ALU.add,
                      [T("Sf"), T("E0"), T("osq")], [T("Sf")])
        b.cpa(Sb, Sf, [T("Sf")], [T("Sb")])
        if not own:
            return
        nk = blk + 1
        NK = nk * 128
        b.act(wab[:, 0:4], k1s[:, 464:468], AF.Abs, [T("k1s")], [T("wab")], scale=0.0625)
        b.act(sgn, k1s[:, 464:468], AF.Sign, [T("k1s")], [T("sgn")])
        for h in range(4):
            b.ts(sd[:, h, :], ident, sgn[:, h:h + 1], None, ALU.mult, None, [T("sgn"), "ident"], [T("sd")])
        b.tt(iqs.rearrange("p (h e) -> p h e", e=64), q1s[:, 256:512].rearrange("p (h e) -> p h e", e=64),
             wab[:, 0:4].unsqueeze(2).to_broadcast([128, 4, 64]), ALU.mult, [T("q1s"), T("wab")], [T("iqs")])
        rope(iqs.rearrange("p (h e) -> p h e", e=64), iqb.rearrange("p (h e) -> p h e", e=64), 4, blk, [T("iqs")], [T("iqb")])
        rope(q2s.rearrange("p (h e) -> p h e", e=64), bqb.rearrange("p (h e) -> p h e", e=64), 8, blk, [T("q2s")], [T("bqb")])
        for c2 in range(2):
            b.tr(ptb[:, c2, :], iqb[:, c2 * 128:(c2 + 1) * 128], ident, [T("iqb")], ["P2"])
        for c4 in range(4):
            b.tr(ptb[:, 2 + c4, :], bqb[:, c4 * 128:(c4 + 1) * 128], ident, [T("bqb")], ["P2"])
        for h2_ in range(2):
            b.ts(iqz[:, h2_, :, :], ptb[:, 0:2, :], rmk[:, h2_:h2_ + 1], None, ALU.mult, None, ["P2", "rowmask"], [T("iqT")])
            b.ts(bqz[h2_], ptb[:, 2:6, :], rmk[:, h2_:h2_ + 1], None, ALU.mult, None, ["P2", "rowmask"], [T("bqT%d" % (blk % 2))])
        b.S.dve(lambda e: e.tensor_reduce(out=bis[:, 0:1], in_=wab[:, 0:4], axis=AX.X, op=ALU.add), [T("wab")], [T("bis")])
        b.ts(bis[:, 1:2], bis[:, 0:1], 64.0, 1e-3, ALU.mult, ALU.add, [T("bis")], [T("bis")])
        b.ts(stp, pw, bis[:, 1:2], None, ALU.mult, None, [T("pw"), T("bis")], [T("stp")])
        b.ts(stp2, stp, 2.0, None, ALU.mult, None, [T("stp")], [T("stp2")])
        ntile = (nk + 3) // 4
        for kt in range(ntile):
            nkb = min(4, nk - 4 * kt)
            N = 128 * nkb
            c0 = kt * 512
            ikkeys = [T("ikT_%d" % j) for j in range(kt * 4, kt * 4 + nkb)]
            for rnd in range(2):
                for h2 in range(2):
                    h = 2 * rnd + h2
                    rs = slice(h2 * 64, (h2 + 1) * 64)
                    pb = ((PA[:, 0:512], "P0"), (PA[:, 512:1024], "P1"), (PC[:, 0:512], "P4"), (PC[:, 512:1024], "P5"))[h]
                    b.mm(pb[0][:, 0:N], iqz[:, h2, rnd, :], kkT[:, 1 - h2, c0:c0 + N], True, True, [T("iqT")] + ikkeys, [pb[1]])
                    b.act(Rr[h][:, 0:N], pb[0][:, 0:N], AF.Relu, [pb[1]], [T("Rr%d" % h)])
            sP, spk = (PB[:, 512:1024], "P3") if kt % 2 == 0 else (PD[:, 0:512], "P6")
            last_tile = kt == ntile - 1
            for h in range(4):
                b.mm(sP[:, 0:N], sd[:, h, :], Rr[h][:, 0:N], h == 0, (h == 3) and not last_tile, [T("sd"), T("Rr%d" % h)], [spk])
            if last_tile:
                off = (nkb - 1) * 128
                b.mm(sP[:, off:off + 128], ident, cb, False, True, [T("cb"), "ident"], [spk])
            b.tt(scr[:, c0:c0 + N].rearrange("p (a t) -> p a t", t=128), sP[:, 0:N].rearrange("p (a t) -> p a t", t=128),
                 kbias[:, kt * 4:kt * 4 + nkb].unsqueeze(2).to_broadcast([128, nkb, 128]), ALU.add, [spk, T("kbias")], [T("scr")])
    def part_B(blk):
        nk = blk + 1
        NK = nk * 128
        mb = mb2[blk % 2]
        jb = mb
        b.memset(mid[0], 0.0, [T("mid0")])
        for it in range(NBIS):
            mi, mo = it % 2, (it + 1) % 2
            b.ts(jb[:, 0:NK], scr[:, 0:NK], mid[mi][:, 0:1], None, ALU.is_gt, ALU.add, [T("scr"), T("mid%d" % mi)],
                 [T("mb%d" % (blk % 2)), T("cnt")], accum_out=cnt[:, 0:1])
            b.ts(dd, cnt, 255.5, stp2[:, it:it + 1], ALU.is_gt, ALU.mult, [T("cnt"), T("stp2")], [T("dd")])
            b.stt(mid[mo], dd, stp[:, it:it + 1], mid[mi], ALU.subtract, ALU.add, [T("dd"), T("stp"), T("mid%d" % mi)],
                  [T("mid%d" % mo)])
        mf = NBIS % 2
        b.tt(bis[:, 2:3], mid[mf], stp[:, NBIS:NBIS + 1], ALU.subtract, [T("mid%d" % mf), T("stp")], [T("bis2")])
        b.ts(mb[:, 0:NK], scr[:, 0:NK], bis[:, 2:3], NEG, ALU.is_le, ALU.mult, [T("scr"), T("bis2")], [T("mb%d" % (blk % 2))])
    def part_C(blk):
        nk = blk + 1
        s = blk % 2
        sx = blk % 3
        xk = T("xt%d" % sx)
        mb = mb2[s]
        cat = cat2[s]
        bqz = bqz2[s]
        Ov = PA.rearrange("p (two x) -> p two x", two=2)[:, :, 0:260].rearrange("p two (h e) -> p two h e", e=65)
        bq0 = [bqz[0].rearrange("p c t -> p (c t)"), bqz[1].rearrange("p c t -> p (c t)")]
        for kb in range(nk):
            ks = slice(kb * 128, (kb + 1) * 128)
            STt = (PC, "P4", "P5") if kb % 2 == 0 else (PA, "P0", "P1")
            ST = STt[0]
            b.mm(ST[:, 0:512], kkT[:, 0, ks], bq0[0], True, False, [T("bkT_%d" % kb), T("bqT%d" % (blk % 2))], [STt[1]])
            b.mm(ST[:, 0:512], mb[:, ks], i4.rearrange("p c t -> p (c t)"), False, True, [T("mb%d" % (blk % 2)), T("i4")], [STt[1]])
            b.mm(ST[:, 512:1024], kkT[:, 1, ks], bq0[1], True, False, [T("bkT_%d" % kb), T("bqT%d" % (blk % 2))], [STt[2]])
            b.mm(ST[:, 512:1024], mb[:, ks], i4.rearrange("p c t -> p (c t)"), False, True, [T("mb%d" % (blk % 2)), T("i4")], [STt[2]])
            pt_ = PT[kb % 2]
            b.act(pt_, ST, AF.Exp, [STt[1], STt[2]], [T("PT%d" % (kb % 2))], scale=0.125)
            for half in range(2):
                b.mm(PD[0:65, half * 512:(half + 1) * 512], bva[:, kb, 0:65], pt_[:, half * 512:(half + 1) * 512], kb == 0, kb == nk - 1,
                     [T("PT%d" % (kb % 2)), T("bva_%d" % kb)], ["P6" if half == 0 else "P7"])
        OTs = junk[0:65, :]
        b.cpa(OTs, PD[0:65, :], ["P6", "P7"], [T("junk")])
        for hh in range(8):
            h2, c4 = hh // 4, hh % 4
            head = 2 * c4 + h2
            b.tr(Ov[:, head // 4, head % 4, :], OTs[:, hh * 128:(hh + 1) * 128], identf[0:65, 0:65], [T("junk")],
                 ["P0" if head < 4 else "P1"])
        for bank in range(2):
            pk = "P0" if bank == 0 else "P1"
            b.ts(den[:, bank * 4:(bank + 1) * 4], Ov[:, bank, :, 64], 1e-30, None, ALU.add, None, [pk], [T("den")])
        b.recip(den, den, [T("den")], [T("den")])
        for bank in range(2):
            pk = "P0" if bank == 0 else "P1"
            b.tt(cat[:, 512 + bank * 256:512 + (bank + 1) * 256].rearrange("p (h e) -> p h e", e=64), Ov[:, bank, :, 0:64],
                 den[:, bank * 4:(bank + 1) * 4].unsqueeze(2).to_broadcast([128, 4, 64]), ALU.mult, [pk, T("den")], [T("cat%d" % (blk % 2))])
        for k in range(8):
            b.tr(ptb[:, k, :], cat[:, k * 128:(k + 1) * 128], ident, [T("cat%d" % (blk % 2))], ["P2"])
        b.cpa(catT, ptb, ["P2"], [T("catT")])
        for half in range(2):
            for k in range(8):
                b.mm(PA[:, half * 512:(half + 1) * 512], catT[:, k, :], wo[:, k, half * 512:(half + 1) * 512], k == 0, k == 7,
                     [T("catT"), T("wo")], ["P0" if half == 0 else "P1"])
        b.tt(xt[sx], PA, xt[sx], ALU.add, ["P0", "P1", xk], [xk])
        ob = blk - first_own
        b.dma("sp", dst[ob * 128:(ob + 1) * 128, :], xt[sx], [xk], ["%s_%d" % (dk, ob)])


    prev = None
    for blk in range(nblocks):
        part_A(blk)
        if blk >= first_own:
            part_B(blk)
        if prev is not None:
            part_C(prev)
        prev = blk if blk >= first_own else None
    if prev is not None:
        part_C(prev)


WEIGHT_SPECS = [
    ("attn_norm", [1, D]), ("attn_w_in", [D, 2516]), ("gla_w_a2", [16, 256]), ("gla_b_a", [1, 256]),
    ("gla_head_g", [1, 128]), ("attn_w_o", [D, D]), ("sgu_norm", [1, D]), ("sgu_w_uv", [D, 2048]),
    ("sgu_ln_g", [1, D]), ("sgu_ln_b", [1, D]), ("sgu_w_s", [8, 128, 128]), ("sgu_b_s", [8, 128]),
    ("sgu_w_out", [D, D]), ("ffn_norm", [2, D]), ("ffn_w_up", [2, D, 2 * DFF]), ("ffn_conv_w", [2, 3, 2 * DFF]),
    ("ffn_conv_b", [2, 2 * DFF]), ("ffn_w_down", [2, DFF, D]), ("final_norm", [1, D]),
]
CONST_SPECS = [
    ("identc", [128, 128]), ("tril", [128, 128]), ("triu", [128, 128]), ("gU", [128, 128]), ("gM1", [128, 128]),
    ("gLs", [128, 128]), ("cbias", [128, 128]), ("kbias", [128, NB]), ("pos", [128, NB]), ("invf", [1, 8]),
    ("pw", [1, NBIS + 1]), ("halo", [128, 1]), ("rowmask", [128, 4]),
]


def host_consts(p):
    i = np.arange(128)
    c = {}
    c["identc"] = np.eye(128, dtype=np.float32)
    c["tril"] = (i[None, :] <= i[:, None]).astype(np.float32)
    c["triu"] = (i[:, None] <= i[None, :]).astype(np.float32)
    sc = np.float32(-1.0 / 16.0)
    U = (i[:, None] <= i[None, :]).astype(np.float32)
    c["gU"] = U * sc
    c["gM1"] = (U - U[:, 64:65]) * sc
    c["gLs"] = (i[:, None] > i[None, :]).astype(np.float32) * sc
    c["cbias"] = np.where(i[None, :] <= i[:, None], 0.0, NEG).astype(np.float32)
    kb = np.zeros((128, NB), np.float32)
    lpos = np.arange(NB * 128)
    if p == 0:
        kb[:, :16] = NEG
        gpos = np.maximum(lpos - 16 * 128, 0)
    else:
        gpos = lpos
    c["kbias"] = kb
    c["pos"] = gpos.reshape(NB, 128).T.astype(np.float32).copy()
    c["invf"] = np.power(np.float32(500000.0), -(np.arange(8, dtype=np.float32) * np.float32(2.0) / np.float32(16.0))).astype(np.float32)[None, :]
    pw = np.array([2.0 ** -(k + 1) for k in range(NBIS)] + [2.0 ** -NBIS], np.float32)
    c["pw"] = pw[None, :]
    c["halo"] = np.full((128, 1), float(p), np.float32)
    rm = np.zeros((128, 4), np.float32)
    rm[:64, 0] = 1.0
    rm[64:, 1] = 1.0
    rm[:64, 2] = 0.125
    rm[64:, 3] = 0.125
    c["rowmask"] = rm
    return c


def build(stages=("l0", "ffn0", "sgu", "ffn1"), l0_first_own=OWN0, l0_nblocks=NB):
    nc = bass.Bass("TRN2", target_bir_lowering=False)
    C = {}
    for name, shape in WEIGHT_SPECS + CONST_SPECS:
        C[name] = nc.dram_tensor(name, shape, F32, kind="ExternalInput").ap()
    for name in ("ffn_w_up", "ffn_conv_w", "ffn_conv_b", "ffn_w_down"):
        t = C[name]
        C[name] = [t[0], t[1]]
    t = C["ffn_norm"]
    C["ffn_norm"] = [t[0:1, :], t[1:2, :]]
    b = B(nc)
    full = tuple(stages) == ("l0", "ffn0", "sgu", "ffn1")
    if stages[0] == "l0":
        xin = nc.dram_tensor("xloc", [NB * 128, D], F32, kind="ExternalInput").ap()
    else:
        xin = nc.dram_tensor("xs_in", [NOWN * 128, D], F32, kind="ExternalInput").ap()
    if stages[-1] == "ffn1":
        out = nc.dram_tensor("out", [16 * 128, D], F32, kind="ExternalOutput").ap()
    else:
        out = nc.dram_tensor("xs_out", [NOWN * 128, D], F32, kind="ExternalOutput").ap()
    for nm in ("PA", "PB", "PC", "PD"):
        C[nm] = b.ps(nm, [128, 1024], F32)
    C["identf"] = b.sb("identf", [128, 128], F32)
    b.dma("sp", C["identf"], C["identc"], [], ["identf"])
    C["ident"] = b.sb("ident", [128, 128], BF16)
    b.cpv(C["ident"], C["identf"], ["identf"], ["ident"])
    trl = b.sb("tril", [128, 128], F32)
    b.dma("sp", trl, C["tril"], [], ["tril"])
    C["tril"] = trl
    hl = b.sb("halo", [128, 1], F32)
    b.dma("sp", hl, C["halo"], [], ["halo"])
    C["halo"] = hl
    rmk = b.sb("rowmask", [128, 4], F32)
    b.dma("sp", rmk, C["rowmask"], [], ["rowmask"])
    C["rowmask"] = rmk
    b.persist()
    cur, curk = xin, "d_in"
    for si, sname in enumerate(stages):
        lastst = si == len(stages) - 1
        dstt = out if lastst else nc.dram_tensor("xs_%s" % sname, [NOWN * 128, D], F32).ap()
        dk = "d_%s" % sname
        b.new_stage()
        if sname == "l0":
            stage_l0(b, C, cur, dstt, dk, first_own=l0_first_own, nblocks=l0_nblocks)
        elif sname == "ffn0":
            stage_ffn(b, C, 0, cur, dstt, 1, 17, False, "f0", curk, dk)
        elif sname == "sgu":
            stage_sgu(b, C, cur, dstt, curk, dk)
        elif sname == "ffn1":
            stage_ffn(b, C, 1, cur, dstt, 2, 16, True, "f1", curk, dk)
        cur, curk = dstt, dk
    counts = b.S.emit()
    global LAST_B
    LAST_B = b
    return nc, counts


def core_inputs(inputs, c):
    bb, p = c // 2, c % 2
    m = {}
    for name, shape in WEIGHT_SPECS:
        m[name] = np.ascontiguousarray(np.asarray(inputs[name], dtype=np.float32).reshape(shape))
    m.update(host_consts(p))
    return m, bb, p


_NC_CACHE = {}


def kernel(**inputs):
    x = np.asarray(inputs["x"], dtype=np.float32)
    if "full" not in _NC_CACHE:
        _NC_CACHE["full"] = build()[0]
    nc = _NC_CACHE["full"]
    in_maps = []
    for c in range(8):
        m, bb, p = core_inputs(inputs, c)
        xl = np.zeros((NB * 128, D), np.float32)
        if p == 1:
            xl[:] = x[bb]
        else:
            xl[16 * 128:] = x[bb, :16 * 128]
        m["xloc"] = xl
        in_maps.append(m)
    res = run_bass_kernel_spmd(nc, in_maps, core_ids=list(range(8)))
    out = np.zeros((4, 4096, D), np.float32)
    for c in range(8):
        bb, p = c // 2, c % 2
        out[bb, p * 2048:(p + 1) * 2048] = res.results[c]["out"]
    return out
```
